# Optimizing a Trainium2 kernel written in Bass

```python
import math
import jax, jax.numpy as jnp
from jax import lax
import numpy as np

D_MODEL = 2048
BATCH = 8
SEQ = 4096
DEPTH = 4

MIX_WIDTH = D_MODEL
GROUP_WIDTH = MIX_WIDTH // 4

RET_HEADS = 4
RET_DK = GROUP_WIDTH // (2 * RET_HEADS)
RET_DV = 2 * RET_DK
RET_CHUNK = 128

SSD_INNER = GROUP_WIDTH
SSD_HEADDIM = 64
SSD_HEADS = SSD_INNER // SSD_HEADDIM
SSD_GROUPS = 2
SSD_STATE = 128
SSD_CONV = 4
SSD_CHUNK = 128
SSD_CONV_DIM = SSD_INNER + 2 * SSD_GROUPS * SSD_STATE
DT_MIN = 1e-3
DT_MAX = 1e-1

DIFF_HEADS = 4
DIFF_HD = GROUP_WIDTH // (2 * DIFF_HEADS)
DIFF_QBLOCK = 128

DIL_HEADS = 8
DIL_HD = GROUP_WIDTH // DIL_HEADS
DIL_PATTERNS = ((128, 1), (512, 4), (2048, 16))

FFN_HIDDEN = -(-8 * D_MODEL // (3 * 256)) * 256

IN_SPLITS = (
    RET_HEADS * RET_DK,
    RET_HEADS * RET_DK,
    RET_HEADS * RET_DV,
    GROUP_WIDTH,
    SSD_INNER,
    SSD_CONV_DIM,
    SSD_HEADS,
    2 * DIFF_HEADS * DIFF_HD,
    2 * DIFF_HEADS * DIFF_HD,
    2 * DIFF_HEADS * DIFF_HD,
    DIL_HEADS * DIL_HD,
    DIL_HEADS * DIL_HD,
    DIL_HEADS * DIL_HD,
)
IN_COLS = sum(IN_SPLITS)
NORM_EPS = 1e-6

kernel_name = 'hybrid_parallel_head_group_block'


def rms_norm(x, g, eps=NORM_EPS):
    xf = x.astype(jnp.float32)
    y = xf * lax.rsqrt(jnp.mean(xf * xf, axis=-1, keepdims=True) + eps)
    return (y * g.astype(jnp.float32)).astype(x.dtype)


def alibi_slopes(n):
    return jnp.exp2(-8.0 * (jnp.arange(n, dtype=jnp.float32) + 1.0) / n)


def causal_depthwise_conv(x, w, b):
    K = w.shape[1]
    y = lax.conv_general_dilated(
        x, jnp.transpose(w)[:, None, :].astype(x.dtype), window_strides=(1,),
        padding=((K - 1, 0),), dimension_numbers=('NWC', 'WIO', 'NWC'),
        feature_group_count=x.shape[-1])
    return y + b.astype(x.dtype)


def retention(q, k, v, gain):
    Bsz, S, H, dk = q.shape
    dv = v.shape[-1]
    C = RET_CHUNK
    n = S // C
    dt = q.dtype
    log_gamma = jnp.log1p(-jnp.exp2(-5.0 - jnp.arange(H, dtype=jnp.float32)))
    pos = jnp.arange(C, dtype=jnp.float32)
    rel = pos[:, None] - pos[None, :]
    intra = jnp.where(rel >= 0, jnp.exp(log_gamma[:, None, None] * jnp.maximum(rel, 0.0)), 0.0).astype(dt)
    zeta = jnp.exp(log_gamma[None, :] * (C - 1.0 - pos)[:, None]).astype(dt)
    xi = jnp.exp(log_gamma[None, :] * (pos + 1.0)[:, None]).astype(dt)
    chunk_decay = jnp.exp(log_gamma * C).astype(dt)
    q = q.reshape(Bsz, n, C, H, dk)
    k = (k * (dk ** -0.5)).reshape(Bsz, n, C, H, dk)
    v = v.reshape(Bsz, n, C, H, dv)
    scores = jnp.einsum('bnihd,bnjhd->bnhij', q, k) * intra
    y_intra = jnp.einsum('bnhij,bnjhe->bnihe', scores, v)
    kv = jnp.einsum('bnjhd,jh,bnjhe->bnhde', k, zeta, v)

    def step(state, kv_c):
        return state * chunk_decay[None, :, None, None] + kv_c, state

    state0 = jnp.zeros((Bsz, H, dk, dv), dt)
    _, prev = lax.scan(step, state0, jnp.moveaxis(kv, 1, 0))
    prev = jnp.moveaxis(prev, 0, 1)
    y_cross = jnp.einsum('bnihd,bnhde->bnihe', q, prev) * xi[None, None, :, :, None]
    y = (y_intra + y_cross).reshape(Bsz, S, H, dv)
    return rms_norm(y, gain.reshape(H, dv))


def ssd_chunked_scan(xdt, a, bm, cm):
    Bsz, S, H, P = xdt.shape
    G, N = bm.shape[2], bm.shape[3]
    R = H // G
    L = SSD_CHUNK
    n = S // L
    dt = xdt.dtype
    x = xdt.reshape(Bsz, n, L, G, R, P)
    bm = bm.reshape(Bsz, n, L, G, N)
    cm = cm.reshape(Bsz, n, L, G, N)
    a = a.astype(jnp.float32).reshape(Bsz, n, L, G, R).transpose(0, 3, 4, 1, 2)
    a_cum = jnp.cumsum(a, axis=-1)
    causal = jnp.tril(jnp.ones((L, L), dtype=bool))
    seg = a_cum[..., :, None] - a_cum[..., None, :]
    decay_in = jnp.where(causal, jnp.exp(jnp.where(causal, seg, 0.0)), 0.0).astype(dt)
    cb = jnp.einsum('bclgn,bcsgn->bgcls', cm, bm)
    y_diag = jnp.einsum('bgcls,bgrcls,bcsgrp->bclgrp', cb, decay_in, x)
    decay_to_end = jnp.exp(a_cum[..., -1:] - a_cum).astype(dt)
    states = jnp.einsum('bcsgn,bgrcs,bcsgrp->bcgrpn', bm, decay_to_end, x)
    chunk_decay = jnp.exp(a_cum[..., -1]).astype(dt)

    def step(h, inp):
        st, dec = inp
        return h * dec[..., None, None] + st, h

    h0 = jnp.zeros((Bsz, G, R, P, N), dt)
    _, h_prev = lax.scan(step, h0, (jnp.moveaxis(states, 1, 0), jnp.moveaxis(chunk_decay, -1, 0)))
    h_prev = jnp.moveaxis(h_prev, 0, 1)
    decay_from_start = jnp.exp(a_cum).astype(dt)
    y_off = jnp.einsum('bclgn,bcgrpn,bgrcl->bclgrp', cm, h_prev, decay_from_start)
    return (y_diag + y_off).reshape(Bsz, S, H, P)


def diff_attention(q, k, v, lam, sub_gain, lam_init, slopes):
    Bsz, S, H, _, d = q.shape
    QB = DIFF_QBLOCK
    nb = S // QB
    scale = d ** -0.5
    q_blocks = q.reshape(Bsz, nb, QB, H, 2, d).transpose(1, 0, 2, 3, 4, 5)
    kpos = jnp.arange(S)

    def one_block(args):
        qb, bi = args
        qpos = bi * QB + jnp.arange(QB)
        dist = qpos[:, None] - kpos[None, :]
        bias = -slopes[:, None, None] * dist.astype(jnp.float32)
        s = jnp.einsum('bqhcd,bkhcd->bhcqk', qb, k).astype(jnp.float32) * scale + bias[None, :, None]
        s = jnp.where(dist >= 0, s, -jnp.inf)
        p = jax.nn.softmax(s, axis=-1)
        att = (p[:, :, 0] - lam * p[:, :, 1]).astype(v.dtype)
        return jnp.einsum('bhqk,bkhe->bqhe', att, v)

    out = lax.map(one_block, (q_blocks, jnp.arange(nb)))
    out = out.transpose(1, 0, 2, 3, 4).reshape(Bsz, S, H, 2 * d)
    return rms_norm(out, sub_gain) * (1.0 - lam_init)


def dilated_branch(q, k, v, slopes, window, dilation):
    Bsz, S, H, d = q.shape
    W = window // dilation
    unit = W * dilation
    Sp = -(-S // unit) * unit
    M = Sp // dilation
    nb = M // W

    def strided(t):
        t = jnp.pad(t, ((0, 0), (0, Sp - S), (0, 0), (0, 0)))
        return t.reshape(Bsz, M, dilation, H, d).transpose(0, 2, 1, 3, 4).reshape(Bsz, dilation, nb, W, H, d)

    def with_prev(t):
        prev = jnp.pad(t, ((0, 0), (0, 0), (1, 0), (0, 0), (0, 0), (0, 0)))[:, :, :-1]
        return jnp.concatenate([prev, t], axis=3)

    qs = strided(q)
    kk = with_prev(strided(k))
    vv = with_prev(strided(v))
    i = jnp.arange(W)
    j = jnp.arange(2 * W)
    steps = W + i[:, None] - j[None, :]
    first = jnp.arange(nb)[:, None, None] == 0
    valid = (steps >= 0) & (steps <= W) & ~(first & (j < W)[None, None, :])
    bias = -slopes[:, None, None] * (steps * dilation).astype(jnp.float32)
    s = jnp.einsum('brnqhd,brnkhd->brnhqk', qs, kk).astype(jnp.float32) * (d ** -0.5) + bias[None, None, None]
    s = jnp.where(valid[None, None, :, None], s, -jnp.inf)
    m = jnp.max(s, axis=-1, keepdims=True)
    p = jnp.exp(s - m)
    l = jnp.sum(p, axis=-1, keepdims=True)
    o = jnp.einsum('brnhqk,brnkhd->brnqhd', (p / l).astype(v.dtype), vv)
    lse = (m + jnp.log(l))[..., 0]
    o = o.reshape(Bsz, dilation, M, H, d).transpose(0, 2, 1, 3, 4).reshape(Bsz, Sp, H, d)[:, :S]
    lse = lse.transpose(0, 1, 2, 4, 3).reshape(Bsz, dilation, M, H).transpose(0, 2, 1, 3).reshape(Bsz, Sp, H)[:, :S]
    return o, lse


def dilated_attention(q, k, v, slopes):
    outs = []
    lses = []
    for window, dilation in DIL_PATTERNS:
        o, lse = dilated_branch(q, k, v, slopes, window, dilation)
        outs.append(o)
        lses.append(lse)
    wts = jax.nn.softmax(jnp.stack(lses, axis=0), axis=0)
    return jnp.einsum('pbsh,pbshd->bshd', wts.astype(q.dtype), jnp.stack(outs, axis=0))


def hybrid_mixer(h, layer, w_in, ret_norm, conv_w, conv_b, dt_bias, a_log, d_skip, ssd_norm_g, diff_lambda, diff_norm_g):
    Bsz, S, _ = h.shape
    split_points = np.cumsum(IN_SPLITS)[:-1].tolist()
    (r_q, r_k, r_v, r_g, s_z, s_xbc, s_dt, d_q, d_k, d_v, l_q, l_k, l_v) = jnp.split(h @ w_in, split_points, axis=-1)

    ret = retention(r_q.reshape(Bsz, S, RET_HEADS, RET_DK), r_k.reshape(Bsz, S, RET_HEADS, RET_DK),
                    r_v.reshape(Bsz, S, RET_HEADS, RET_DV), ret_norm)
    out_a = ret.reshape(Bsz, S, RET_HEADS * RET_DV) * jax.nn.silu(r_g)

    xbc = jax.nn.silu(causal_depthwise_conv(s_xbc, conv_w, conv_b))
    s_x, s_b, s_c = jnp.split(xbc, [SSD_INNER, SSD_INNER + SSD_GROUPS * SSD_STATE], axis=-1)
    s_x = s_x.reshape(Bsz, S, SSD_HEADS, SSD_HEADDIM)
    dt = jax.nn.softplus((s_dt + dt_bias).astype(jnp.float32))
    a = dt * (-jnp.exp(a_log.astype(jnp.float32)))
    y = ssd_chunked_scan(s_x * dt[..., None].astype(s_x.dtype), a,
                         s_b.reshape(Bsz, S, SSD_GROUPS, SSD_STATE), s_c.reshape(Bsz, S, SSD_GROUPS, SSD_STATE))
    y = y + s_x * d_skip[:, None].astype(s_x.dtype)
    y = (y.reshape(Bsz, S, SSD_INNER) * jax.nn.silu(s_z)).reshape(Bsz, S, SSD_GROUPS, SSD_INNER // SSD_GROUPS)
    out_b = rms_norm(y, ssd_norm_g.reshape(SSD_GROUPS, SSD_INNER // SSD_GROUPS)).reshape(Bsz, S, SSD_INNER)

    lam_init = 0.8 - 0.6 * math.exp(-0.3 * layer)
    lf = diff_lambda.astype(jnp.float32)
    lam = jnp.exp(jnp.sum(lf[0] * lf[1])) - jnp.exp(jnp.sum(lf[2] * lf[3])) + lam_init
    out_c = diff_attention(d_q.reshape(Bsz, S, DIFF_HEADS, 2, DIFF_HD), d_k.reshape(Bsz, S, DIFF_HEADS, 2, DIFF_HD),
                           d_v.reshape(Bsz, S, DIFF_HEADS, 2 * DIFF_HD), lam, diff_norm_g, lam_init,
                           alibi_slopes(DIFF_HEADS)).reshape(Bsz, S, 2 * DIFF_HEADS * DIFF_HD)

    out_d = dilated_attention(l_q.reshape(Bsz, S, DIL_HEADS, DIL_HD), l_k.reshape(Bsz, S, DIL_HEADS, DIL_HD),
                              l_v.reshape(Bsz, S, DIL_HEADS, DIL_HD), alibi_slopes(DIL_HEADS)).reshape(Bsz, S, DIL_HEADS * DIL_HD)

    return jnp.concatenate([out_a, out_b, out_c, out_d], axis=-1)


def setup_inputs(seed: int = 0) -> dict:
    key = jax.random.key(seed)
    ks = jax.random.split(key, 20)
    f32 = jnp.float32

    def nrm(k, shape, scale):
        return scale * jax.random.normal(k, shape, f32)

    def gain(k, shape):
        return 1.0 + 0.02 * jax.random.normal(k, shape, f32)

    x = jax.random.normal(ks[0], (BATCH, SEQ, D_MODEL), f32)
    norm_mix_pre = gain(ks[1], (DEPTH, D_MODEL))
    w_in = nrm(ks[2], (DEPTH, D_MODEL, IN_COLS), D_MODEL ** -0.5)
    ret_norm = gain(ks[3], (DEPTH, RET_HEADS * RET_DV))
    ssd_conv_w = nrm(ks[4], (DEPTH, SSD_CONV_DIM, SSD_CONV), SSD_CONV ** -0.5)
    ssd_conv_b = nrm(ks[5], (DEPTH, SSD_CONV_DIM), 0.02)
    dt0 = jnp.exp(jax.random.uniform(ks[6], (DEPTH, SSD_HEADS), f32, math.log(DT_MIN), math.log(DT_MAX)))
    ssd_dt_bias = dt0 + jnp.log(-jnp.expm1(-dt0))
    ssd_a_log = jnp.log(jax.random.uniform(ks[7], (DEPTH, SSD_HEADS), f32, 1.0, 16.0))
    ssd_d = gain(ks[8], (DEPTH, SSD_HEADS))
    ssd_norm = gain(ks[9], (DEPTH, SSD_INNER))
    diff_lambda = nrm(ks[10], (DEPTH, 4, DIFF_HD), 0.1)
    diff_norm = gain(ks[11], (DEPTH, 2 * DIFF_HD))
    w_out = nrm(ks[12], (DEPTH, MIX_WIDTH, D_MODEL), MIX_WIDTH ** -0.5)
    norm_mix_post = gain(ks[13], (DEPTH, D_MODEL))
    norm_ffn_pre = gain(ks[14], (DEPTH, D_MODEL))
    w_gate = nrm(ks[15], (DEPTH, D_MODEL, FFN_HIDDEN), D_MODEL ** -0.5)
    w_up = nrm(ks[16], (DEPTH, D_MODEL, FFN_HIDDEN), D_MODEL ** -0.5)
    w_down = nrm(ks[17], (DEPTH, FFN_HIDDEN, D_MODEL), FFN_HIDDEN ** -0.5)
    norm_ffn_post = gain(ks[18], (DEPTH, D_MODEL))
    return {'x': x, 'norm_mix_pre': norm_mix_pre, 'w_in': w_in, 'ret_norm': ret_norm,
            'ssd_conv_w': ssd_conv_w, 'ssd_conv_b': ssd_conv_b, 'ssd_dt_bias': ssd_dt_bias,
            'ssd_a_log': ssd_a_log, 'ssd_d': ssd_d, 'ssd_norm': ssd_norm, 'diff_lambda': diff_lambda,
            'diff_norm': diff_norm, 'w_out': w_out, 'norm_mix_post': norm_mix_post,
            'norm_ffn_pre': norm_ffn_pre, 'w_gate': w_gate, 'w_up': w_up, 'w_down': w_down,
            'norm_ffn_post': norm_ffn_post}


def reference(x, norm_mix_pre, w_in, ret_norm, ssd_conv_w, ssd_conv_b, ssd_dt_bias, ssd_a_log, ssd_d,
              ssd_norm, diff_lambda, diff_norm, w_out, norm_mix_post, norm_ffn_pre, w_gate, w_up, w_down,
              norm_ffn_post):
    for l in range(DEPTH):
        h = rms_norm(x, norm_mix_pre[l])
        mix = hybrid_mixer(h, l, w_in[l], ret_norm[l], ssd_conv_w[l], ssd_conv_b[l], ssd_dt_bias[l],
                           ssd_a_log[l], ssd_d[l], ssd_norm[l], diff_lambda[l], diff_norm[l])
        x = x + rms_norm(mix @ w_out[l], norm_mix_post[l])
        h = rms_norm(x, norm_ffn_pre[l])
        f = (jax.nn.silu(h @ w_gate[l]) * (h @ w_up[l])) @ w_down[l]
        x = x + rms_norm(f, norm_ffn_post[l])
    return x
```

```python
import math
from contextlib import ExitStack
import numpy as np
import ml_dtypes
import concourse.bass as bass
import concourse.mybir as mybir
from concourse.bass_utils import run_bass_kernel_spmd

F32 = mybir.dt.float32
BF16 = mybir.dt.bfloat16
AF = mybir.ActivationFunctionType
ALU = mybir.AluOpType
AX = mybir.AxisListType

S = 4096
D = 2048
NL = 4
INC = 6152
HID = 5632
EPS = 1e-6
NEG = -262144.0
NT = S // 128
TP = 512
NPASS = S // TP
DBG = {}


def DS(start, count, step=1):
    return slice(start, start + (count - 1) * step + 1, step)


class Sem:
    def __init__(self, h):
        self.h = h
        self.total = 0


class Buf:
    def __init__(self, name):
        self.name = name
        self.writes = {}
        self.reads = {}
        self.dsem = None
        self.psum = False


class Tile:
    def __init__(self, t, buf):
        self.t = t
        self.buf = buf

    def __getitem__(self, k):
        return self.t[k]


class Eng:
    def __init__(self, h, sem, kind):
        self.h = h
        self.sem = sem
        self.known = {}
        self.kind = kind


def _b(x):
    return x.buf if isinstance(x, Tile) else x


class Prog:
    def __init__(self, nc, es):
        self.nc = nc
        self.es = es
        self.allsems = []
        self.free_dsems = []

        def mk(h, kind):
            s = self.newsem()
            return Eng(h, s, kind)

        self.pe = mk(nc.tensor, "pe")
        self.act = mk(nc.scalar, "act")
        self.dve = mk(nc.vector, "dve")
        self.pool = mk(nc.gpsimd, "pool")
        self.sp = mk(nc.sync, "sp")
        self.engs = [self.pe, self.act, self.dve, self.pool, self.sp]
        self.castsems = [self.newsem() for _ in range(8)]
        self.castk = 0
        self.nid = 0
        self.dq = []

    def newsem(self):
        h = self.es.enter_context(self.nc.semaphore(f"s{len(self.allsems)}"))
        s = Sem(h)
        self.allsems.append(s)
        return s

    def dsem(self, buf):
        if buf.dsem is None:
            buf.dsem = self.free_dsems.pop() if self.free_dsems else self.newsem()
        return buf.dsem

    def _wait(self, eng, deps):
        for sem, val in deps.items():
            if val > eng.known.get(sem, 0):
                eng.h.wait_ge(sem.h, val)
                eng.known[sem] = val

    def op(self, eng, fn, reads=(), writes=(), signal=True):
        reads = [_b(x) for x in reads]
        writes = [_b(x) for x in writes]
        deps = {}

        def add(sem, val, raw):
            if sem is eng.sem:
                if not raw or eng.kind == "pe":
                    return
            if val > deps.get(sem, 0):
                deps[sem] = val

        for b in reads:
            for sem, val in b.writes.items():
                add(sem, val, True)
            if b.psum:
                for sem, val in b.reads.items():
                    add(sem, val, False)
        for b in writes:
            for sem, val in b.writes.items():
                add(sem, val, False)
            for sem, val in b.reads.items():
                add(sem, val, False)
        self._wait(eng, deps)
        ins = fn()
        cnt = eng.sem.total + 1
        if signal:
            ins.then_inc(eng.sem.h, 1)
            eng.sem.total = cnt
        for b in reads:
            if cnt > b.reads.get(eng.sem, 0):
                b.reads[eng.sem] = cnt
        for b in writes:
            if cnt > b.writes.get(eng.sem, 0):
                b.writes[eng.sem] = cnt
        return ins

    def dma(self, q, out, in_, reads=(), writes=(), sembuf=None, sem=None):
        reads = [_b(x) for x in reads]
        writes = [_b(x) for x in writes]
        if sem is None:
            sem = self.dsem(_b(sembuf))
        deps = {}
        for b in reads:
            for s_, v in b.writes.items():
                deps[s_] = max(deps.get(s_, 0), v)
        for b in writes:
            for s_, v in b.reads.items():
                deps[s_] = max(deps.get(s_, 0), v)
            for s_, v in b.writes.items():
                if s_ is not sem:
                    deps[s_] = max(deps.get(s_, 0), v)
        self._wait(q, deps)
        q.h.dma_start(out=out, in_=in_).then_inc(sem.h, 16)
        sem.total += 16
        for b in reads:
            b.reads[sem] = sem.total
        for b in writes:
            b.writes[sem] = sem.total

    def dstore(self, *a, **kw):
        self.dq.append((a, kw))

    def dflush_for(self, tile, keep=1):
        b = _b(tile)
        if any(b in [_b(x) for x in kw.get("reads", ())] for a, kw in self.dq):
            self.dflush(0)
        else:
            self.dflush(keep)

    def dflush(self, keep=0):
        while len(self.dq) > keep:
            a, kw = self.dq.pop(0)
            self.dma(*a, **kw)

    def barrier(self):
        self.dflush(0)
        for e in self.engs:
            for s in self.allsems:
                if s.total > e.known.get(s, 0):
                    e.h.wait_ge(s.h, s.total)
                    e.known[s] = s.total

    def mm(self, out, lhsT, rhs, start, stop, reads, writes, signal=True):
        nc = self.nc
        return self.op(self.pe, lambda: nc.tensor.matmul(out, lhsT, rhs, start=start, stop=stop),
                       reads, writes, signal)

    def actf(self, out, in_, func, reads, writes, bias=None, scale=1.0, accum_out=None):
        nc = self.nc
        kw = {}
        if bias is not None:
            kw["bias"] = bias
        if accum_out is not None:
            kw["accum_out"] = accum_out
        return self.op(self.act, lambda: nc.scalar.activation(out=out, in_=in_, func=func, scale=scale, **kw),
                       reads, writes)

    def v(self, fn, reads, writes):
        return self.op(self.dve, fn, reads, writes)


class Scope:
    def __init__(self, P):
        self.P = P
        self.es = ExitStack()
        self.tiles = []

    def __enter__(self):
        self.es.__enter__()
        return self

    def tile(self, name, shape, dt):
        P = self.P
        P.nid += 1
        t = self.es.enter_context(P.nc.sbuf_tensor(f"{name}_{P.nid}", list(shape), dt))
        T = Tile(t, Buf(name))
        self.tiles.append(T)
        return T

    def __exit__(self, *a):
        self.P.barrier()
        for T in self.tiles:
            if T.buf.dsem is not None:
                self.P.free_dsems.append(T.buf.dsem)
                T.buf.dsem = None
        return self.es.__exit__(*a)


class Pref:
    def __init__(self, n, bufs, loadfn, dist):
        self.n, self.bufs, self.loadfn, self.dist, self.issued = n, bufs, loadfn, dist, 0

    def get(self, i):
        while self.issued < min(self.n, i + self.dist + 1):
            j = self.issued
            self.loadfn(j, self.bufs[j % len(self.bufs)])
            self.issued += 1
        return self.bufs[i % len(self.bufs)]


class Rot:
    def __init__(self, items):
        self.items = items
        self.i = 0

    def next(self):
        x = self.items[self.i % len(self.items)]
        self.i += 1
        return x


CF = {}
CB = {}


def _build_consts():
    cf = []
    cb = []

    def addf(name, arr):
        arr = np.asarray(arr, np.float32)
        assert arr.shape[0] == 128
        c0 = sum(a.shape[1] for a in cf)
        CF[name] = (c0, arr.shape[1])
        cf.append(arr)

    def addb(name, arr):
        arr = np.asarray(arr, np.float32)
        assert arr.shape[0] == 128
        c0 = sum(a.shape[1] for a in cb)
        CB[name] = (c0, arr.shape[1])
        cb.append(arr)

    idx = np.arange(128)
    addf("eps", np.full((128, 1), EPS))
    addf("one", np.ones((128, 1)))
    addf("tri", (idx[:, None] <= idx[None, :]).astype(np.float32))
    addf("onesf", np.ones((128, 128)))
    addf("mean128", np.full((128, 128), 1.0 / 128))
    addf("mean256", np.full((128, 64), 1.0 / 256))
    nm = np.where(idx[None, :] >= idx[:, None], 0.0, -30000.0)
    addf("negmask4", np.tile(nm, (1, 4)))
    for h in range(4):
        lg = math.log1p(-2.0 ** (-5.0 - h))
        rel = idx[None, :] - idx[:, None]
        rm = np.where(rel >= 0, np.exp(lg * np.maximum(rel, 0)), 0.0) * (64 ** -0.5)
        addf(f"rmask{h}", np.tile(rm, (1, 4)))
        addf(f"xi{h}", np.tile(np.exp(lg * (idx + 1.0))[None, :], (128, 1)))
        addf(f"zeta{h}", (np.exp(lg * (127.0 - idx)) * (64 ** -0.5))[:, None])
    addb("ident", np.eye(128))
    addb("ones", np.ones((128, 128)))
    md = np.zeros((128, 512))
    md[:, :128] = np.where(idx[:, None] > idx[None, :], NEG, 0.0)
    addb("mdiag", md)
    ii = np.arange(256)
    band = np.where((idx[:, None] <= ii[None, :]) & (ii[None, :] <= idx[:, None] + 128), 0.0, NEG)
    addb("band", band)
    cfa = np.concatenate(cf, 1).astype(np.float32)
    cba = np.concatenate(cb, 1).astype(ml_dtypes.bfloat16)
    t = np.arange(S)
    aug = np.zeros((12, 2, 4, S), np.float32)
    slopes = [2.0 ** (-8.0 * (h + 1) / 4) for h in range(4)] + [2.0 ** (-8.0 * (h + 1) / 8) for h in range(8)]
    for a, s in enumerate(slopes):
        aug[a, 0, 0] = 8 * s * (t % 128)
        aug[a, 0, 1] = 8 * s * 128 * (t // 128)
        aug[a, 0, 2] = 1
        aug[a, 0, 3] = 1
        aug[a, 1, 0] = 1
        aug[a, 1, 1] = 1
        aug[a, 1, 2] = -8 * s * (t % 128)
        aug[a, 1, 3] = -8 * s * 128 * (t // 128)
    return cfa, cba, aug.astype(ml_dtypes.bfloat16)


CFA, CBA, AUG = _build_consts()

PPC = {}


def _pack_params(inp):
    cols = []

    def add(name, arr):
        c0 = sum(a.shape[2] for a in cols)
        PPC[name] = (c0, arr.shape[2])
        cols.append(np.ascontiguousarray(arr, dtype=np.float32))

    add("retn", inp["ret_norm"].reshape(NL, 4, 128).transpose(0, 2, 1))
    add("convw", inp["ssd_conv_w"].reshape(NL, 8, 128, 4).transpose(0, 2, 1, 3).reshape(NL, 128, 32))
    add("convb", inp["ssd_conv_b"].reshape(NL, 8, 128).transpose(0, 2, 1))
    add("dtb", np.broadcast_to(inp["ssd_dt_bias"][:, None, :], (NL, 128, 8)))
    add("alog", np.broadcast_to(inp["ssd_a_log"][:, None, :], (NL, 128, 8)))
    add("dsk", np.broadcast_to(inp["ssd_d"][:, None, :], (NL, 128, 8)))
    sn = inp["ssd_norm"].reshape(NL, 8, 64).transpose(0, 2, 1)
    add("ssdn", np.concatenate([sn, sn], 1))
    add("lam", np.broadcast_to(inp["diff_lambda"].reshape(NL, 1, 256), (NL, 128, 256)))
    add("difn", inp["diff_norm"].reshape(NL, 128, 1))
    return np.concatenate(cols, 2)


A_BLOCKS = [
    (0, 512, "F", "r_qkT", 0, "copy"),
    (256, 256, "T", "r_ktm", 0, "copy"),
    (512, 512, "T", "r_vtm", 0, "copy"),
    (1024, 512, "F", "r_gT", 0, "silu"),
    (1536, 512, "F", "s_zT", 0, "silu"),
    (2048, 512, "F", "s_xbcT", 0, "copy"),
    (2560, 512, "F", "s_xbcT", 512, "copy"),
    (3072, 8, "T", "s_dt", 0, "copy32"),
    (3080, 512, "F", "d_qT", 0, "copy"),
    (3592, 512, "F", "d_kT", 0, "copy"),
    (4104, 512, "T", "d_vtm", 0, "copy"),
    (4616, 512, "F", "l_qT", 0, "copy"),
    (5128, 512, "F", "l_kT", 0, "copy"),
    (5640, 512, "T", "l_vtm", 0, "copy"),
]
SCR = {
    "r_qkT": ([512, S], BF16), "r_ktm": ([S, 256], BF16), "r_vtm": ([S, 512], BF16),
    "r_gT": ([512, S], BF16), "s_zT": ([512, S], BF16), "s_xbcT": ([1024, S], BF16),
    "s_dt": ([S, 8], F32), "d_qT": ([512, S], BF16), "d_kT": ([512, S], BF16),
    "d_vtm": ([S, 512], BF16), "l_qT": ([512, S], BF16), "l_kT": ([512, S], BF16),
    "l_vtm": ([S, 512], BF16), "mixT": ([D, S], BF16),
}


class K:
    def __init__(self, layers, debug=False, phases="ABCD", mixers="abcd"):
        self.layers = layers
        self.debug = debug
        self.phases = phases
        self.mixers = mixers
        self.nc = bass.Bass("TRN2", target_bir_lowering=False)
        self.build()

    def dram_in(self, name, shape, dt):
        return self.nc.dram_tensor(name, list(shape), dt, kind="ExternalInput").ap()

    def build(self):
        nc = self.nc
        self.x_in = self.dram_in("x", [S, D], F32)
        if not DBG.get("now"):
            self.w_in = self.dram_in("w_in", [NL, D, INC], F32)
            self.w_out = self.dram_in("w_out", [NL, D, D], F32)
            self.w_gate = self.dram_in("w_gate", [NL, D, HID], F32)
            self.w_up = self.dram_in("w_up", [NL, D, HID], F32)
            self.w_down = self.dram_in("w_down", [NL, HID, D], F32)
        self.gn = [self.dram_in(n, [NL, D], F32) for n in ("norm_mix_pre", "norm_mix_post", "norm_ffn_pre", "norm_ffn_post")]
        self.pp = self.dram_in("pp", [NL, 128, sum(v[1] for v in PPC.values())], F32)
        self.cfa = self.dram_in("cfa", list(CFA.shape), F32)
        self.cba = self.dram_in("cba", list(CBA.shape), BF16)
        self.aug = self.dram_in("aug", list(AUG.shape), BF16)
        self.out = nc.dram_tensor("out", [S, D], F32, kind="ExternalOutput").ap()
        if DBG.get('dbgw'):
            self.dbgw = nc.dram_tensor("dbgw", [128, 4096], BF16, kind="ExternalOutput").ap()
            self.dbgwb = Buf('dbgw')
        kind = "ExternalOutput" if self.debug else "Internal"
        self.scr = {}
        self.scrb = {}
        for n, (shp, dt) in SCR.items():
            if DBG.get("now"):
                kind = "ExternalOutput" if n == "mixT" else "ExternalInput"
            self.scr[n] = nc.dram_tensor("scr_" + n, shp, dt, kind=kind).ap()
            self.scrb[n] = Buf("scr_" + n)
        self.outb = Buf("out")
        self.outbs = [Buf(f"out{i}") for i in range(NT)]
        self.wq = {}
        self.wqb = {}
        for l in ([] if DBG.get("now") else self.layers):
            tot = sum(b[1] for b in A_BLOCKS)
            self.wq[("in", l)] = nc.dram_tensor(f"wq_in{l}", [128, 16 * tot], BF16, kind="Internal").ap()
            self.wq[("out", l)] = nc.dram_tensor(f"wq_out{l}", [128, 16 * D], BF16, kind="Internal").ap()
            self.wq[("gate", l)] = nc.dram_tensor(f"wq_gate{l}", [128, 16 * HID], BF16, kind=("ExternalOutput" if DBG.get("dbgw") else "Internal")).ap()
            self.wq[("up", l)] = nc.dram_tensor(f"wq_up{l}", [128, 16 * HID], BF16, kind="Internal").ap()
            self.wq[("down", l)] = nc.dram_tensor(f"wq_down{l}", [128, 44 * D], BF16, kind="Internal").ap()
        with ExitStack() as es:
            self.P = P = Prog(nc, es)
            with Scope(P) as g:
                self.g = g
                self.cf = g.tile("cf", list(CFA.shape), F32)
                self.cb = g.tile("cb", list(CBA.shape), BF16)
                P.dma(P.sp, self.cf[:], self.cfa[:], writes=[self.cf], sembuf=self.cf)
                P.dma(P.sp, self.cb[:], self.cba[:], writes=[self.cb], sembuf=self.cb)
                self.ps = []
                for i in range(8):
                    t = es.enter_context(nc.psum_tensor(f"psb{i}", [128, 512], F32))
                    self.ps.append(Tile(t, Buf(f"ps{i}")))
                    self.ps[-1].buf.psum = True
                self.cast_plan = []
                self.plan_casts()
                self.emit_casts(len(A_BLOCKS))
                first = True
                for l in self.layers:
                    xsrc, xsb = (self.x_in, None) if first else (self.out, self.outb)
                    if "A" in self.phases:
                        self.phase_A(l, xsrc, xsb)
                    if "B" in self.phases:
                        self.phase_B(l)
                    if "C" in self.phases:
                        self.phase_C(l, xsrc, xsb)
                    if "D" in self.phases:
                        self.phase_D(l)
                    first = False
                self.emit_casts(10 ** 9)
            P.barrier()

    def cfc(self, name, rows=128):
        c0, n = CF[name]
        return self.cf[0:rows, c0:c0 + n]

    def cbc(self, name, rows=128):
        c0, n = CB[name]
        return self.cb[0:rows, c0:c0 + n]

    def ppc(self, T, name):
        c0, n = PPC[name]
        return T[:, c0:c0 + n]

    def plan_casts(self):
        for l in ([] if DBG.get("now") else self.layers):
            off = 0
            for bi, (c0, n, *_r) in enumerate(A_BLOCKS):
                src = self.w_in[l, :, c0:c0 + n].rearrange("(k p) c -> p k c", p=128)
                dst = self.wq[("in", l)][:, off:off + 16 * n].rearrange("p (k c) -> p k c", k=16)
                b = Buf(f"wqin{l}_{bi}")
                self.wqb[("in", l, bi)] = (dst, b)
                b.cast_idx = len(self.cast_plan)
                self.cast_plan.append((src, dst, b))
                off += 16 * n
            for cbk in range(4):
                src = self.w_out[l, :, cbk * 512:(cbk + 1) * 512].rearrange("(k p) c -> p k c", p=128)
                dst = self.wq[("out", l)][:, cbk * 8192:(cbk + 1) * 8192].rearrange("p (k c) -> p k c", k=16)
                b = Buf("wqo")
                self.wqb[("out", l, cbk)] = (dst, b)
                b.cast_idx = len(self.cast_plan)
                self.cast_plan.append((src, dst, b))
            for nm, W in (("gate", self.w_gate), ("up", self.w_up)):
                for cbk in range(22):
                    src = W[l, :, cbk * 256:(cbk + 1) * 256].rearrange("(k p) c -> p k c", p=128)
                    dst = self.wq[(nm, l)][:, cbk * 4096:(cbk + 1) * 4096].rearrange("p (k c) -> p k c", k=16)
                    b = Buf("wqg")
                    self.wqb[(nm, l, cbk)] = (dst, b)
                    b.cast_idx = len(self.cast_plan)
                    self.cast_plan.append((src, dst, b))
            for cbk in range(4):
                for kg in range(4):
                    src = self.w_down[l, kg * 1408:(kg + 1) * 1408, cbk * 512:(cbk + 1) * 512].rearrange("(k p) c -> p k c", p=128)
                    o = (cbk * 4 + kg) * 11 * 512
                    dst = self.wq[("down", l)][:, o:o + 11 * 512].rearrange("p (k c) -> p k c", k=11)
                    b = Buf("wqd")
                    self.wqb[("down", l, cbk, kg)] = (dst, b)
                    b.cast_idx = len(self.cast_plan)
                    self.cast_plan.append((src, dst, b))
        self.cast_i = 0

    def ensure_cast(self, b):
        if self.cast_i <= b.cast_idx:
            self.emit_casts(b.cast_idx - self.cast_i + 1)

    def emit_casts(self, n):
        P = self.P
        while n > 0 and self.cast_i < len(self.cast_plan):
            src, dst, b = self.cast_plan[self.cast_i]
            sem = P.castsems[P.castk % len(P.castsems)]
            P.castk += 1
            if sem.total > P.pool.known.get(sem, 0):
                P.pool.h.wait_ge(sem.h, sem.total)
                P.pool.known[sem] = sem.total
            P.dma(P.pool, dst, src, writes=[b], sem=sem)
            self.cast_i += 1
            n -= 1

    def rstd_from_ssq(self, ssq, lnv, rstd, scale):
        P = self.P
        P.actf(lnv[:], ssq[:], AF.Ln, [ssq, self.cf], [lnv], bias=self.cfc("eps"), scale=scale)
        P.actf(rstd[:], lnv[:], AF.Exp, [lnv], [rstd], scale=-0.5)

    def norm_part(self, X, Hb, gT, small):
        P, nc = self.P, self.nc
        ssq, lnv, rstd = small
        P.actf(Hb[:], X[:], AF.Square, [X], [Hb, ssq], accum_out=ssq[:])
        self.rstd_from_ssq(ssq, lnv, rstd, 1.0 / D)
        P.v(lambda: nc.vector.scalar_tensor_tensor(Hb[:], X[:], rstd[:], gT[:], ALU.mult, ALU.mult),
            [X, rstd, gT], [Hb])

    def trans_part(self, Hb, HT, tt, psrot, ei):
        P, nc = self.P, self.nc
        ident = self.cbc("ident")
        for k4 in range(4):
            ps = psrot.next()
            psb = ps[:].bitcast(BF16)
            for kk in range(4):
                k = k4 * 4 + kk
                P.op(P.pe, lambda: nc.tensor.transpose(psb[:, kk * 128:(kk + 1) * 128], Hb[:, k * 128:(k + 1) * 128], ident),
                     [Hb, self.cb], [ps], signal=(kk == 3))
            dst = HT[:, k4 * 4:(k4 + 1) * 4, tt * 128:(tt + 1) * 128]
            src = psb[:, 0:512].rearrange("p (a b) -> p a b", a=4)
            if (ei + k4) % 2 == 0:
                P.op(P.act, lambda: nc.scalar.copy(out=dst, in_=src), [ps], [HT])
            else:
                P.v(lambda: nc.vector.tensor_copy(dst, src), [ps], [HT])

    def norm_transpose(self, X, Hb, gT, HT, tt, small, psrot, ei):
        self.norm_part(X, Hb, gT, small)
        self.trans_part(Hb, HT, tt, psrot, ei)

    def load_g(self, T, which, l):
        P = self.P
        P.dma(P.sp, T[:], self.gn[which][l:l + 1, :].broadcast_to([128, D]), writes=[T], sembuf=T)

    def post_norm_residual(self, Y, X, gT, small, dst_rows):
        P, nc = self.P, self.nc
        ssq, lnv, rstd = small
        P.actf(self.junk[:], Y[:], AF.Square, [Y], [self.junk, ssq], accum_out=ssq[:])
        self.rstd_from_ssq(ssq, lnv, rstd, 1.0 / D)
        P.v(lambda: nc.vector.scalar_tensor_tensor(Y[:], Y[:], rstd[:], gT[:], ALU.mult, ALU.mult), [Y, rstd, gT], [Y])
        P.v(lambda: nc.vector.tensor_tensor(X[:], X[:], Y[:], ALU.add), [X, Y], [X])
        P.dstore(P.act, self.out[dst_rows, :], X[:], reads=[X], writes=[self.outbs[dst_rows.start // 128]], sembuf=X)

    def phase_A(self, l, xsrc, xsb):
        P, nc = self.P, self.nc
        with Scope(P) as sc:
            xt = Rot([sc.tile("xt", [128, D], F32) for _ in range(2)])
            hbs = [sc.tile("hb", [128, D], BF16) for _ in range(4)]
            hT = [sc.tile("hT", [128, 16, TP], BF16) for _ in range(2)]
            wbufs = [sc.tile("wb", [128, 16, 512], BF16) for _ in range(3)]
            stg = Rot([sc.tile("stg", [128, 512], BF16) for _ in range(4)])
            stg32 = Rot([sc.tile("stg32", [128, 8], F32) for _ in range(2)])
            g1 = sc.tile("g1", [128, D], F32)
            smalls = Rot([[sc.tile("sm", [128, 1], F32) for _ in range(3)] for _ in range(2)])
            psrot = Rot(self.ps)
            self.load_g(g1, 0, l)
            nb = len(A_BLOCKS)

            def loadw(j, W):
                bi = j % nb
                n = A_BLOCKS[bi][1]
                wsrc, wbuf = self.wqb[("in", l, bi)]
                self.ensure_cast(wbuf)
                P.dma(P.sp, W[:, :, 0:n], wsrc, reads=[wbuf], writes=[W], sembuf=W)

            wp = Pref(NPASS * nb, wbufs, loadw, 2)
            self.ei = 0

            def do_norm_a(tp):
                for tt in range(4):
                    t0 = tp * TP + tt * 128
                    X = xt.next()
                    P.dma(P.sp, X[:], xsrc[t0:t0 + 128, :], reads=[self.outbs[t0 // 128]] if xsb else [], writes=[X], sembuf=X)
                    self.norm_part(X, hbs[tt], g1, smalls.next())

            def do_norm_b(tp):
                for tt in range(4):
                    self.trans_part(hbs[tt], hT[tp % 2], tt, psrot, self.ei)
                    self.ei += 1

            do_norm_a(0)
            do_norm_b(0)
            for tp in range(NPASS):
                self.emit_casts(12 if l == self.layers[0] else 0)
                HT = hT[tp % 2]
                for bi, (c0, n, orient, dname, doff, func) in enumerate(A_BLOCKS):
                    W = wp.get(tp * nb + bi)
                    if bi == 8 and tp + 1 < NPASS:
                        do_norm_a(tp + 1)
                    if bi == 10 and tp + 1 < NPASS:
                        do_norm_b(tp + 1)
                    dst = self.scr[dname]
                    dbuf = self.scrb[dname]
                    if orient == "F":
                        for cc in range(n // 128):
                            ps = psrot.next()
                            for k in range(16):
                                P.mm(ps[:, :], W[:, k, cc * 128:(cc + 1) * 128], HT[:, k, :], k == 0, k == 15,
                                     [W, HT], [ps], signal=(k == 15))
                            st = stg.next()
                            P.dflush(2)
                            if func == "silu":
                                P.actf(st[:], ps[:], AF.Silu, [ps], [st])
                            elif self.ei % 2 == 0:
                                P.op(P.act, lambda: nc.scalar.copy(out=st[:], in_=ps[:]), [ps], [st])
                            else:
                                P.v(lambda: nc.vector.tensor_copy(st[:], ps[:]), [ps], [st])
                            self.ei += 1
                            r0 = doff + cc * 128
                            P.dstore(P.act, dst[r0:r0 + 128, tp * TP:(tp + 1) * TP], st[:], reads=[st], writes=[dbuf], sembuf=st)
                    else:
                        for tt in range(4):
                            ps = psrot.next()
                            for k in range(16):
                                P.mm(ps[:, 0:n], HT[:, k, tt * 128:(tt + 1) * 128], W[:, k, 0:n], k == 0, k == 15,
                                     [W, HT], [ps], signal=(k == 15))
                            st = stg32.next() if func == "copy32" else stg.next()
                            P.dflush(1 if func == "copy32" else 2)
                            if self.ei % 2 == 0:
                                P.op(P.act, lambda: nc.scalar.copy(out=st[:, 0:n], in_=ps[:, 0:n]), [ps], [st])
                            else:
                                P.v(lambda: nc.vector.tensor_copy(st[:, 0:n], ps[:, 0:n]), [ps], [st])
                            self.ei += 1
                            t0 = tp * TP + tt * 128
                            P.dstore(P.act, dst[t0:t0 + 128, doff:doff + n], st[:, 0:n], reads=[st], writes=[dbuf], sembuf=st)
            P.dflush(0)

    def phase_C(self, l, xsrc, xsb):
        P, nc = self.P, self.nc
        mixT = self.scr["mixT"].rearrange("(k p) s -> p k s", p=128)
        with Scope(P) as sc:
            mbufs = [sc.tile("mt", [128, 16, TP], BF16) for _ in range(2)]
            wbufs = [sc.tile("wb", [128, 16, 512], BF16) for _ in range(3)]
            Y = [sc.tile("Y", [128, D], F32) for _ in range(4)]
            xt = Rot([sc.tile("xt", [128, D], F32) for _ in range(2)])
            self.junk = sc.tile("junk", [128, D], BF16)
            g2 = sc.tile("g2", [128, D], F32)
            smalls = Rot([[sc.tile("sm", [128, 1], F32) for _ in range(3)] for _ in range(2)])
            psrot = Rot(self.ps)
            self.load_g(g2, 1, l)

            def loadw(j, W):
                wsrc, wbuf = self.wqb[("out", l, j % 4)]
                self.ensure_cast(wbuf)
                P.dma(P.sp, W[:], wsrc, reads=[wbuf], writes=[W], sembuf=W)

            def loadm(j, M):
                P.dma(P.sp, M[:], mixT[:, :, j * TP:(j + 1) * TP], reads=[self.scrb["mixT"]], writes=[M], sembuf=M)

            wp = Pref(NPASS * 4, wbufs, loadw, 2)
            mp = Pref(NPASS, mbufs, loadm, 1)
            ei = 0
            for tp in range(NPASS):
                self.emit_casts(1)
                M = mp.get(tp)
                for cbk in range(4):
                    W = wp.get(tp * 4 + cbk)
                    for tt in range(4):
                        ps = psrot.next()
                        for k in range(16):
                            P.mm(ps[:, :], M[:, k, tt * 128:(tt + 1) * 128], W[:, k, :], k == 0, k == 15, [W, M], [ps], signal=(k == 15))
                        ysl = Y[tt][:, cbk * 512:(cbk + 1) * 512]
                        if ei % 2 == 0:
                            P.op(P.act, lambda: nc.scalar.copy(out=ysl, in_=ps[:]), [ps], [Y[tt]])
                        else:
                            P.v(lambda: nc.vector.tensor_copy(ysl, ps[:]), [ps], [Y[tt]])
                        ei += 1
                for tt in range(4):
                    t0 = tp * TP + tt * 128
                    X = xt.next()
                    P.dflush_for(X)
                    P.dma(P.sp, X[:], xsrc[t0:t0 + 128, :], reads=[self.outbs[t0 // 128]] if xsb else [], writes=[X], sembuf=X)
                    self.post_norm_residual(Y[tt], X, g2, smalls.next(), slice(t0, t0 + 128))
            P.dflush(0)

    def phase_D(self, l):
        P, nc = self.P, self.nc
        with Scope(P) as sc:
            xt = Rot([sc.tile("xt", [128, D], F32) for _ in range(2)])
            hbs = [sc.tile("hb", [128, D], BF16) for _ in range(2)]
            hbt = hbs[0]
            self.junk = hbt
            hT = [sc.tile("hT", [128, 16, TP], BF16) for _ in range(2)]
            wbufs = [sc.tile("wb", [128, 32, 128], BF16) for _ in range(2)]
            wdbufs = [sc.tile("wd", [128, 11, 512], BF16) for _ in range(2)]
            actT = sc.tile("actT", [128, 44, TP], BF16)
            Fo = [sc.tile("F", [128, D], F32) for _ in range(4)]
            sg = Rot([sc.tile("sg", [128, 512], F32) for _ in range(2)])
            g3 = sc.tile("g3", [128, D], F32)
            g4 = sc.tile("g4", [128, D], F32)
            smalls = Rot([[sc.tile("sm", [128, 1], F32) for _ in range(3)] for _ in range(2)])
            psrot = Rot(self.ps)
            self.load_g(g3, 2, l)
            self.load_g(g4, 3, l)

            def loadgu(j, W):
                hb2, cc = divmod(j % 44, 2)
                s1, b1 = self.wqb[("gate", l, hb2)]
                s2, b2 = self.wqb[("up", l, hb2)]
                self.ensure_cast(b1)
                self.ensure_cast(b2)
                if DBG.get('castbar'):
                    P.barrier()
                P.dma(P.sp, W[:, 0:16, :], s1[:, :, cc * 128:(cc + 1) * 128], reads=[b1], writes=[W], sembuf=W)
                P.dma(P.sp, W[:, 16:32, :], s2[:, :, cc * 128:(cc + 1) * 128], reads=[b2], writes=[W], sembuf=W)
                if DBG.get('dbgw') and j == DBG['dbgw'] - 1:
                    P.dma(P.sp, self.dbgw, W[:].rearrange('p k c -> p (k c)'), reads=[W], writes=[self.dbgwb], sembuf=W)

            def loadwd(j, W):
                jj = j % 16
                s1, b1 = self.wqb[("down", l, jj // 4, jj % 4)]
                self.ensure_cast(b1)
                P.dma(P.sp, W[:], s1, reads=[b1], writes=[W], sembuf=W)

            gup = Pref(NPASS * 44, wbufs, loadgu, 1)
            wdp = Pref(NPASS * 16, wdbufs, loadwd, 1)
            self.ei = 0

            def npart(tp, tt):
                t0 = tp * TP + tt * 128
                X = xt.next()
                P.dflush_for(X)
                P.dma(P.sp, X[:], self.out[t0:t0 + 128, :], reads=[self.outbs[t0 // 128]], writes=[X], sembuf=X)
                self.norm_part(X, hbs[tt % 2], g3, smalls.next())

            def tpart(tp, tt):
                self.trans_part(hbs[tt % 2], hT[tp % 2], tt, psrot, self.ei)
                self.ei += 1

            def do_norm_early(tp):
                npart(tp, 0)
                npart(tp, 1)

            def do_norm_late(tp):
                tpart(tp, 0)
                npart(tp, 2)
                tpart(tp, 1)
                npart(tp, 3)
                tpart(tp, 2)
                tpart(tp, 3)

            def do_norm(tp):
                do_norm_early(tp)
                do_norm_late(tp)

            P.dflush(0)
            do_norm(0)
            for tp in range(NPASS):
                self.emit_casts(1)
                if DBG.get('noearly') and tp > 0:
                    do_norm(tp)
                HT = hT[tp % 2]
                for hc in range(44):
                    W = gup.get(tp * 44 + hc)
                    psg = psrot.next()
                    psu = psrot.next()
                    for k in range(16):
                        P.mm(psg[:, :], W[:, k, :], HT[:, k, :], k == 0, k == 15, [W, HT], [psg], signal=(k == 15))
                    for k in range(16):
                        P.mm(psu[:, :], W[:, 16 + k, :], HT[:, k, :], k == 0, k == 15, [W, HT], [psu], signal=(k == 15))
                    sgt = sg.next()
                    P.actf(sgt[:], psg[:], AF.Silu, [psg], [sgt])
                    P.v(lambda: nc.vector.tensor_tensor(actT[:, hc, :], sgt[:], psu[:], ALU.mult), [sgt, psu], [actT])
                for cbk in range(4):
                    banks = [psrot.next() for _ in range(4)]
                    if cbk == 1 and tp + 1 < NPASS and not DBG.get('noearly'):
                        do_norm_early(tp + 1)
                    for kg in range(4):
                        Wd = wdp.get(tp * 16 + cbk * 4 + kg)
                        for tt in range(4):
                            for k in range(11):
                                P.mm(banks[tt][:, :], actT[:, kg * 11 + k, tt * 128:(tt + 1) * 128], Wd[:, k, :],
                                     kg == 0 and k == 0, kg == 3 and k == 10, [Wd, actT], [banks[tt]], signal=(k == 10))
                    for tt in range(4):
                        fsl = Fo[tt][:, cbk * 512:(cbk + 1) * 512]
                        if self.ei % 2 == 0:
                            P.op(P.act, lambda: nc.scalar.copy(out=fsl, in_=banks[tt][:]), [banks[tt]], [Fo[tt]])
                        else:
                            P.v(lambda: nc.vector.tensor_copy(fsl, banks[tt][:]), [banks[tt]], [Fo[tt]])
                        self.ei += 1
                    if cbk == 1 and tp + 1 < NPASS and not DBG.get('noearly'):
                        do_norm_late(tp + 1)
                for tt in range(4):
                    t0 = tp * TP + tt * 128
                    X = xt.next()
                    P.dflush_for(X)
                    P.dma(P.sp, X[:], self.out[t0:t0 + 128, :], reads=[self.outbs[t0 // 128]], writes=[X], sembuf=X)
                    self.post_norm_residual(Fo[tt], X, g4, smalls.next(), slice(t0, t0 + 128))
            P.dflush(0)

    def phase_B(self, l):
        P = self.P
        with Scope(P) as sc:
            self.ppt = sc.tile("ppt", [128, self.pp.shape[2]], F32)
            P.dma(P.sp, self.ppt[:], self.pp[l], writes=[self.ppt], sembuf=self.ppt)
            if "a" in self.mixers:
                self.mixer_ret(l)
            if "b" in self.mixers:
                self.mixer_ssd(l)
            if "c" in self.mixers:
                self.mixer_diff(l)
            if "d" in self.mixers:
                self.mixer_dil(l)

    def rms_feat(self, src_tiles, rows, meanname, gain_ap, gain_buf, outs, ps_pool, tmp, mult_tiles=None):
        P, nc = self.P, self.nc
        sqs, lnv, rs = tmp
        psm = ps_pool.next()
        n = len(src_tiles)
        for i, (ap, buf) in enumerate(src_tiles):
            P.actf(sqs[i][0:rows, :], ap, AF.Square, [buf], [sqs[i]])
        for i in range(n):
            P.mm(psm[0:rows, :], self.cfc(meanname, rows)[:, 0:rows], sqs[i][0:rows, :], i == 0, i == n - 1, [sqs[i], self.cf], [psm], signal=(i == n - 1))
        P.actf(lnv[0:rows, :], psm[0:rows, :], AF.Ln, [psm, self.cf], [lnv], bias=self.cfc("eps", rows), scale=1.0)
        P.actf(rs[0:rows, :], lnv[0:rows, :], AF.Exp, [lnv], [rs], scale=-0.5)
        return rs

    def mixer_ret(self, l):
        P, nc = self.P, self.nc
        qk = self.scr["r_qkT"]
        with Scope(P) as sc:
            qT = sc.tile("qT", [64, S], BF16)
            kT = sc.tile("kT", [64, S], BF16)
            qx = sc.tile("qx", [64, S], BF16)
            ktm = sc.tile("ktm", [128, NT, 64], BF16)
            kz = sc.tile("kz", [128, NT, 64], BF16)
            vt = sc.tile("vt", [128, NT, 128], BF16)
            gT = sc.tile("gT", [128, S], BF16)
            prev = sc.tile("prev", [64, NT, 128], BF16)
            Sst = sc.tile("Sst", [64, 128], F32)
            scb = Rot([sc.tile("scb", [128, 512], BF16) for _ in range(2)])
            sq = sc.tile("sq", [128, 512], F32)
            lnv = sc.tile("lnv", [128, 512], F32)
            rs = sc.tile("rs", [128, 512], F32)
            t1 = sc.tile("t1", [128, 512], F32)
            ob = Rot([sc.tile("ob", [128, 512], BF16) for _ in range(2)])
            pool = Rot(self.ps)
            for h in range(4):
                self.emit_casts(3)
                decay = float(np.exp(np.float64(math.log1p(-2.0 ** (-5.0 - h))) * 128))
                P.dma(P.sp, qT[:], qk[h * 64:(h + 1) * 64, :], reads=[self.scrb["r_qkT"]], writes=[qT], sembuf=qT)
                P.dma(P.sp, kT[:], qk[256 + h * 64:256 + (h + 1) * 64, :], reads=[self.scrb["r_qkT"]], writes=[kT], sembuf=kT)
                P.dma(P.sp, ktm[:], self.scr["r_ktm"][:, h * 64:(h + 1) * 64].rearrange("(n p) c -> p n c", p=128),
                      reads=[self.scrb["r_ktm"]], writes=[ktm], sembuf=ktm)
                P.dma(P.sp, vt[:], self.scr["r_vtm"][:, h * 128:(h + 1) * 128].rearrange("(n p) c -> p n c", p=128),
                      reads=[self.scrb["r_vtm"]], writes=[vt], sembuf=vt)
                P.dma(P.sp, gT[:], self.scr["r_gT"][h * 128:(h + 1) * 128, :], reads=[self.scrb["r_gT"]], writes=[gT], sembuf=gT)
                xi = self.cfc(f"xi{h}", 64)
                P.v(lambda: nc.vector.tensor_tensor(qx[:].rearrange("p (n c) -> p n c", c=128), qT[:].rearrange("p (n c) -> p n c", c=128),
                                                    xi.unsqueeze(1).broadcast_to([64, NT, 128]), ALU.mult), [qT, self.cf], [qx])
                P.v(lambda: nc.vector.tensor_scalar(kz[:], ktm[:], self.cfc(f"zeta{h}"), None, ALU.mult), [ktm, self.cf], [kz])
                P.v(lambda: nc.vector.memset(Sst[:], 0.0), [], [Sst])
                for n4 in range(8):
                    ps = pool.next()
                    for j in range(4):
                        n = n4 * 4 + j
                        P.mm(ps[0:64, j * 128:(j + 1) * 128], kz[:, n, :], vt[:, n, :], True, True, [kz, vt], [ps], signal=(j == 3))
                    for j in range(4):
                        n = n4 * 4 + j
                        P.v(lambda: nc.vector.tensor_copy(prev[:, n, :], Sst[:]), [Sst], [prev])
                        P.v(lambda: nc.vector.scalar_tensor_tensor(Sst[:], Sst[:], decay, ps[0:64, j * 128:(j + 1) * 128], ALU.mult, ALU.add),
                            [Sst, ps], [Sst])
                rmask = self.cfc(f"rmask{h}")
                gain = self.ppc(self.ppt, "retn")[:, h:h + 1]
                def front(st):
                    n4 = st["n4"]
                    pss = pool.next()
                    for j in range(4):
                        n = n4 * 4 + j
                        P.mm(pss[:, j * 128:(j + 1) * 128], kT[:, n * 128:(n + 1) * 128], qT[:, n * 128:(n + 1) * 128], True, True,
                             [kT, qT], [pss], signal=(j == 3))
                    scs = scb.next()
                    P.v(lambda: nc.vector.tensor_tensor(scs[:], pss[:], rmask, ALU.mult), [pss, self.cf], [scs])
                    st["scs"] = scs

                def back(st):
                    n4, scs = st["n4"], st["scs"]
                    psy = pool.next()
                    for j in range(4):
                        n = n4 * 4 + j
                        P.mm(psy[:, j * 128:(j + 1) * 128], vt[:, n, :], scs[:, j * 128:(j + 1) * 128], True, False, [vt, scs], [psy], signal=False)
                        P.mm(psy[:, j * 128:(j + 1) * 128], prev[:, n, :], qx[:, n * 128:(n + 1) * 128], False, True, [prev, qx], [psy], signal=(j == 3))
                    rsx = self.rms_feat([(psy[:], psy)], 128, "mean128", None, None, None, pool, ([sq], lnv, rs))
                    P.v(lambda: nc.vector.scalar_tensor_tensor(t1[:], psy[:], gain, rsx[:], ALU.mult, ALU.mult), [psy, self.ppt, rs], [t1])
                    o = ob.next()
                    P.v(lambda: nc.vector.tensor_tensor(o[:], t1[:], gT[:, n4 * 512:(n4 + 1) * 512], ALU.mult), [t1, gT], [o])
                    P.dma(P.sp, self.scr["mixT"][h * 128:(h + 1) * 128, n4 * 512:(n4 + 1) * 512], o[:], reads=[o], writes=[self.scrb["mixT"]], sembuf=o)

                self.pipeline([dict(n4=n4) for n4 in range(8)], front, back, 1)

    @staticmethod
    def pipeline(steps, front, back, look):
        n = len(steps)
        for i in range(min(look, n)):
            front(steps[i])
        for i in range(n):
            if i + look < n:
                front(steps[i + look])
            back(steps[i])

    def mixer_diff(self, l):
        P, nc = self.P, self.nc
        lam_init = 0.8 - 0.6 * math.exp(-0.3 * l)
        with Scope(P) as sc:
            Kc = [[sc.tile("Kc", [68, S], BF16) for _ in range(2)] for _ in range(2)]
            Qc = [[sc.tile("Qc", [68, S], BF16) for _ in range(2)] for _ in range(2)]
            V = [sc.tile("V", [128, NT, 128], BF16) for _ in range(2)]
            PT = Rot([sc.tile("PT", [128, 512], BF16) for _ in range(4)])
            R = [sc.tile("R", [128, 512], F32) for _ in range(2)]
            AB = [sc.tile("AB", [128, 512], F32) for _ in range(2)]
            Dd = sc.tile("Dd", [128, 512], F32)
            sq = sc.tile("sq", [128, 512], F32)
            lnv = sc.tile("lnv", [128, 512], F32)
            rs = sc.tile("rs", [128, 512], F32)
            ob = Rot([sc.tile("ob", [128, 512], BF16) for _ in range(2)])
            lt = sc.tile("lt", [128, 8], F32)
            lp = sc.tile("lp", [128, 128], F32)
            accp = Rot(self.ps[0:4])
            scp = Rot(self.ps[4:8])
            lamv = self.ppc(self.ppt, "lam")
            P.v(lambda: nc.vector.tensor_tensor(lp[:, 0:64], lamv[:, 0:64], lamv[:, 64:128], ALU.mult), [self.ppt], [lp])
            P.v(lambda: nc.vector.tensor_tensor(lp[:, 64:128], lamv[:, 128:192], lamv[:, 192:256], ALU.mult), [self.ppt], [lp])
            P.v(lambda: nc.vector.reduce_sum(lt[:, 0:1], lp[:, 0:64], axis=AX.X), [lp], [lt])
            P.v(lambda: nc.vector.reduce_sum(lt[:, 1:2], lp[:, 64:128], axis=AX.X), [lp], [lt])
            P.actf(lt[:, 2:4], lt[:, 0:2], AF.Exp, [lt], [lt])
            P.v(lambda: nc.vector.tensor_tensor(lt[:, 4:5], lt[:, 3:4], lt[:, 2:3], ALU.subtract), [lt], [lt])
            P.v(lambda: nc.vector.tensor_scalar(lt[:, 5:6], lt[:, 4:5], -lam_init, None, ALU.add), [lt], [lt])
            P.v(lambda: nc.vector.tensor_scalar(lt[:, 6:7], self.ppc(self.ppt, "difn"), 1.0 - lam_init, None, ALU.mult), [self.ppt], [lt])
            neglam = lt[:, 5:6]
            gain = lt[:, 6:7]
            ident = self.cbc("ident")
            ones = self.cbc("ones")
            mdiag = self.cbc("mdiag")
            NH = DBG.get('diff_nh', 4)

            def load_kq(h):
                hp = h % 2
                for c in range(2):
                    r0 = h * 128 + c * 64
                    P.dma(P.sp, Qc[hp][c][0:64, :], self.scr["d_qT"][r0:r0 + 64, :], reads=[self.scrb["d_qT"]], writes=[Qc[hp][c]], sembuf=Qc[hp][c])
                    P.dma(P.sp, Qc[hp][c][64:68, :], self.aug[h, 1], writes=[Qc[hp][c]], sembuf=Qc[hp][c])
                    P.dma(P.sp, Kc[hp][c][0:64, :], self.scr["d_kT"][r0:r0 + 64, :], reads=[self.scrb["d_kT"]], writes=[Kc[hp][c]], sembuf=Kc[hp][c])
                    P.dma(P.sp, Kc[hp][c][64:68, :], self.aug[h, 0], writes=[Kc[hp][c]], sembuf=Kc[hp][c])

            def load_v(h):
                hp = h % 2
                P.dma(P.sp, V[hp][:], self.scr["d_vtm"][:, h * 128:(h + 1) * 128].rearrange("(n p) c -> p n c", p=128),
                      reads=[self.scrb["d_vtm"]], writes=[V[hp]], sembuf=V[hp])

            steps = []
            for h in range(NH):
                for Q in range(8):
                    nJ = 4 * Q + 4
                    grp = {}
                    for J in range(nJ):
                        for c in range(2):
                            steps.append(dict(h=h, Q=Q, J=J, c=c, nJ=nJ, grp=grp, foh=(Q == 0 and J == 0 and c == 0),
                                              fog=(J == 0 and c == 0), log=(J == nJ - 1 and c == 1)))
            load_kq(0)
            load_v(0)

            def front(s):
                h, Q, J, c = s["h"], s["Q"], s["J"], s["c"]
                hp = h % 2
                if s["foh"] and h + 1 < NH:
                    load_kq(h + 1)
                if s["fog"]:
                    self.emit_casts(2)
                r = J - 4 * Q
                c0 = 128 * r if r >= 0 else 0
                ps = scp.next()
                if r >= 0:
                    P.mm(ps[:, c0:512], ident, mdiag[:, 0:512 - c0], True, False, [self.cb], [ps], signal=False)
                P.mm(ps[:, c0:512], Kc[hp][c][0:68, J * 128:(J + 1) * 128], Qc[hp][c][0:68, Q * 512 + c0:(Q + 1) * 512],
                     r < 0, True, [Kc[hp][c], Qc[hp][c]], [ps])
                pt = PT.next()
                P.actf(pt[:, c0:512], ps[:, c0:512], AF.Exp, [ps], [pt], scale=0.125)
                s["pt"] = pt
                s["c0"] = c0

            def back(s):
                h, Q, J, c, nJ, grp = s["h"], s["Q"], s["J"], s["c"], s["nJ"], s["grp"]
                hp = h % 2
                if s["foh"] and h + 1 < NH:
                    load_v(h + 1)
                if s["fog"]:
                    grp["O"] = [accp.next(), accp.next()]
                    grp["L"] = [accp.next(), accp.next()]
                O, Lb = grp["O"], grp["L"]
                pt, c0 = s["pt"], s["c0"]
                P.mm(O[c][:, c0:512], V[hp][:, J, :], pt[:, c0:512], J == 0, J == nJ - 1, [V[hp], pt], [O[c]], signal=False)
                P.mm(Lb[c][:, c0:512], ones, pt[:, c0:512], J == 0, J == nJ - 1, [self.cb, pt], [Lb[c]], signal=True)
                if s["log"]:
                    for cc in range(2):
                        P.v(lambda: nc.vector.reciprocal(R[cc][:], Lb[cc][:]), [Lb[cc]], [R[cc]])
                        P.v(lambda: nc.vector.tensor_tensor(AB[cc][:], O[cc][:], R[cc][:], ALU.mult), [O[cc], R[cc]], [AB[cc]])
                    P.v(lambda: nc.vector.scalar_tensor_tensor(Dd[:], AB[1][:], neglam, AB[0][:], ALU.mult, ALU.add), [AB[0], AB[1], lt], [Dd])
                    rsx = self.rms_feat([(Dd[:], Dd)], 128, "mean128", None, None, None, scp, ([sq], lnv, rs))
                    o = ob.next()
                    P.v(lambda: nc.vector.scalar_tensor_tensor(o[:], Dd[:], gain, rsx[:], ALU.mult, ALU.mult), [Dd, lt, rs], [o])
                    P.dma(P.sp, self.scr["mixT"][1024 + h * 128:1024 + (h + 1) * 128, Q * 512:(Q + 1) * 512], o[:],
                          reads=[o], writes=[self.scrb["mixT"]], sembuf=o)

            self.pipeline(steps, front, back, 3)

    def mixer_dil(self, l):
        P, nc = self.P, self.nc
        pats = (1, 4, 16)
        with Scope(P) as sc:
            Ka = [sc.tile("Ka", [68, S], BF16) for _ in range(2)]
            Qa = [sc.tile("Qa", [68, S], BF16) for _ in range(2)]
            Vp = [sc.tile("Vp", [128, NT, 512], BF16) for _ in range(3)]
            PT = Rot([sc.tile("PT", [128, 256], BF16) for _ in range(4)])
            Ntot = sc.tile("Ntot", [64, S], F32)
            Ltot = sc.tile("Ltot", [64, S], F32)
            ob = sc.tile("ob", [64, S], BF16)
            accp = Rot(self.ps[0:4])
            scp = Rot(self.ps[4:8])
            ident = self.cbc("ident")
            ones = self.cbc("ones")
            band = self.cbc("band")
            vb = self.scrb["l_vtm"]
            for pi, dil in enumerate(pats):
                src = self.scr["l_vtm"].rearrange("(n p r) c -> p r n c", p=128, r=dil)
                dstv = Vp[pi][:].rearrange("p (r n) c -> p r n c", r=dil)
                for r in range(dil):
                    P.dma(P.sp, dstv[:, r], src[:, r], reads=[vb], writes=[Vp[pi]], sembuf=Vp[pi])

            def load_kq(h):
                hp = h % 2
                P.dma(P.sp, Qa[hp][0:64, :], self.scr["l_qT"][h * 64:(h + 1) * 64, :], reads=[self.scrb["l_qT"]], writes=[Qa[hp]], sembuf=Qa[hp])
                P.dma(P.sp, Qa[hp][64:68, :], self.aug[4 + h, 1], writes=[Qa[hp]], sembuf=Qa[hp])
                P.dma(P.sp, Ka[hp][0:64, :], self.scr["l_kT"][h * 64:(h + 1) * 64, :], reads=[self.scrb["l_kT"]], writes=[Ka[hp]], sembuf=Ka[hp])
                P.dma(P.sp, Ka[hp][64:68, :], self.aug[4 + h, 0], writes=[Ka[hp]], sembuf=Ka[hp])

            steps = []
            for h in range(8):
                hsteps = []
                for pi, dil in enumerate(pats):
                    nb = NT // dil
                    for r in range(dil):
                        for g0 in range(0, nb, 4):
                            gsz = min(4, nb - g0)
                            ns = list(range(max(g0 - 1, 0), g0 + gsz))
                            grp = {}
                            for n in ns:
                                hsteps.append(dict(h=h, pi=pi, dil=dil, r=r, g0=g0, gsz=gsz, n=n, nb=nb, grp=grp,
                                                   fog=(n == ns[0]), log=(n == ns[-1]), foh=False, loh=False))
                hsteps[0]["foh"] = True
                hsteps[-1]["loh"] = True
                steps += hsteps
            load_kq(0)

            def front(s):
                h, dil, r, g0, gsz, n = s["h"], s["dil"], s["r"], s["g0"], s["gsz"], s["n"]
                hp = h % 2
                if s["foh"]:
                    self.emit_casts(2)
                if s["foh"] and h + 1 < 8:
                    load_kq(h + 1)
                qbs = [qb for qb in (n, n + 1) if g0 <= qb < g0 + gsz]
                lo = (qbs[0] - n) * 128
                N = 128 * len(qbs)
                ps = scp.next()
                P.mm(ps[:, 0:N], ident, band[:, lo:lo + N], True, False, [self.cb], [ps], signal=False)
                P.mm(ps[:, 0:N], Ka[hp][0:68, DS(n * 128 * dil + r, 128, dil)], Qa[hp][0:68, DS(qbs[0] * 128 * dil + r, N, dil)],
                     False, True, [Ka[hp], Qa[hp]], [ps])
                pt = PT.next()
                P.actf(pt[:, 0:N], ps[:, 0:N], AF.Exp, [ps], [pt], scale=0.125)
                s["pt"] = pt
                s["qbs"] = qbs

            def back(s):
                h, pi, dil, r, g0, gsz, n, nb, grp = s["h"], s["pi"], s["dil"], s["r"], s["g0"], s["gsz"], s["n"], s["nb"], s["grp"]
                if s["fog"]:
                    grp["On"] = accp.next()
                    grp["Ln"] = accp.next()
                On, Ln_ = grp["On"], grp["Ln"]
                pt = s["pt"]
                for qi, qb in enumerate(s["qbs"]):
                    firstc = (n == qb - 1) or (qb == 0)
                    lastc = (n == qb)
                    col = (qb - g0) * 128
                    P.mm(On[0:64, col:col + 128], Vp[pi][:, r * nb + n, h * 64:(h + 1) * 64], pt[:, qi * 128:(qi + 1) * 128],
                         firstc, lastc, [Vp[pi], pt], [On], signal=False)
                    P.mm(Ln_[0:64, col:col + 128], ones[:, 0:64], pt[:, qi * 128:(qi + 1) * 128],
                         firstc, lastc, [self.cb, pt], [Ln_], signal=True)
                if s["log"]:
                    tok = DS(g0 * 128 * dil + r, gsz * 128, dil)
                    W_ = gsz * 128
                    if pi == 0:
                        P.op(P.act, lambda: nc.scalar.copy(out=Ntot[:, tok], in_=On[0:64, 0:W_]), [On], [Ntot])
                        P.v(lambda: nc.vector.tensor_copy(Ltot[:, tok], Ln_[0:64, 0:W_]), [Ln_], [Ltot])
                    else:
                        P.v(lambda: nc.vector.tensor_tensor(Ntot[:, tok], On[0:64, 0:W_], Ntot[:, tok], ALU.add), [On, Ntot], [Ntot])
                        P.v(lambda: nc.vector.tensor_tensor(Ltot[:, tok], Ln_[0:64, 0:W_], Ltot[:, tok], ALU.add), [Ln_, Ltot], [Ltot])
                if s["loh"]:
                    P.v(lambda: nc.vector.reciprocal(Ltot[:], Ltot[:]), [Ltot], [Ltot])
                    P.v(lambda: nc.vector.tensor_tensor(ob[:], Ntot[:], Ltot[:], ALU.mult), [Ntot, Ltot], [ob])
                    P.dma(P.sp, self.scr["mixT"][1536 + h * 64:1536 + (h + 1) * 64, :], ob[:], reads=[ob], writes=[self.scrb["mixT"]], sembuf=ob)

            self.pipeline(steps, front, back, 2)

    def mixer_ssd(self, l):
        P, nc = self.P, self.nc
        ppt = self.ppt
        with Scope(P) as sc:
            z = sc.tile("z", [128, NT, 8], F32)
            az = sc.tile("az", [128, NT, 8], F32)
            dtv = sc.tile("dtv", [128, NT, 8], F32)
            a = sc.tile("a", [128, NT, 8], F32)
            acum = sc.tile("acum", [128, NT, 8], F32)
            dte = sc.tile("dte", [128, NT, 8], F32)
            cdec = sc.tile("cdec", [128, NT, 8], F32)
            wts = sc.tile("wts", [128, NT, 8], F32)
            Aneg = sc.tile("Aneg", [128, 8], F32)
            DI = sc.tile("DI", [128, 8, 128], BF16)
            pool = Rot(self.ps)
            P.dma(P.sp, z[:], self.scr["s_dt"].rearrange("(n p) h -> p n h", p=128), reads=[self.scrb["s_dt"]], writes=[z], sembuf=z)
            if DBG.get('ssd_sub') == 1:
                return
            dtb = self.ppc(ppt, "dtb")
            P.v(lambda: nc.vector.tensor_tensor(z[:], z[:], dtb.unsqueeze(1).broadcast_to([128, NT, 8]), ALU.add), [z, ppt], [z])
            if DBG.get('ssd_sub') == 2:
                return
            P.actf(az[:], z[:], AF.Abs, [z], [az])
            P.actf(az[:], az[:], AF.Exp, [az], [az], scale=-1.0)
            P.actf(az[:], az[:], AF.Ln, [az, self.cf], [az], bias=self.cfc("one"), scale=1.0)
            if DBG.get('ssd_sub') == 3:
                return
            P.v(lambda: nc.vector.scalar_tensor_tensor(dtv[:], z[:], 0.0, az[:], ALU.max, ALU.add), [z, az], [dtv])
            if DBG.get('ssd_sub') == 4:
                return
            P.actf(Aneg[:], self.ppc(ppt, "alog"), AF.Exp, [ppt], [Aneg])
            P.v(lambda: nc.vector.tensor_scalar(Aneg[:], Aneg[:], -1.0, None, ALU.mult), [Aneg], [Aneg])
            P.v(lambda: nc.vector.tensor_tensor(a[:], dtv[:], Aneg[:].unsqueeze(1).broadcast_to([128, NT, 8]), ALU.mult), [dtv, Aneg], [a])
            if DBG.get('ssd_sub') == 5:
                return
            a2 = a[:].rearrange("p n h -> p (n h)")
            ps1 = pool.next()
            P.mm(ps1[:, 0:256], self.cfc("tri"), a2, True, True, [self.cf, a], [ps1])
            P.v(lambda: nc.vector.tensor_copy(acum[:].rearrange("p n h -> p (n h)"), ps1[:, 0:256]), [ps1], [acum])
            if DBG.get('ssd_sub') == 6:
                return
            ps2 = pool.next()
            P.mm(ps2[:, 0:256], self.cfc("onesf"), a2, True, True, [self.cf, a], [ps2])
            P.actf(cdec[:].rearrange("p n h -> p (n h)"), ps2[:, 0:256], AF.Exp, [ps2], [cdec])
            P.v(lambda: nc.vector.tensor_tensor(dte[:].rearrange("p n h -> p (n h)"), ps2[:, 0:256], acum[:].rearrange("p n h -> p (n h)"), ALU.subtract),
                [ps2, acum], [dte])
            P.actf(dte[:], dte[:], AF.Exp, [dte], [dte])
            P.v(lambda: nc.vector.tensor_tensor(wts[:], dtv[:], dte[:], ALU.mult), [dtv, dte], [wts])
            if DBG.get('ssd_sub') == 7:
                return
            dsk = self.ppc(ppt, "dsk")
            for hh in range(8):
                P.v(lambda: nc.vector.tensor_scalar(DI[:, hh, :], self.cbc("ident"), dsk[:, hh:hh + 1], None, ALU.mult), [self.cb, ppt], [DI])
            if DBG.get('ssd_stop') == 1:
                return
            convw = self.ppc(ppt, "convw")
            convb = self.ppc(ppt, "convb")
            for g in range(2):
                self.emit_casts(4)
                with Scope(P) as sg:
                    BT = sg.tile("BT", [128, S], BF16)
                    CT = sg.tile("CT", [128, S], BF16)
                    xtm = sg.tile("xtm", [128, NT, 256], BF16)
                    Btm = sg.tile("Btm", [128, NT, 128], BF16)
                    with Scope(P) as s1:
                        xin = s1.tile("xin", [128, S + 3], BF16)
                        acc = s1.tile("acc", [128, S], F32)
                        xcb = s1.tile("xcb", [128, S], BF16)
                        P.v(lambda: nc.vector.memset(xin[:, 0:3], 0.0), [], [xin])
                        blocks = [(2 * g, "x0"), (2 * g + 1, "x1"), (4 + g, "B"), (6 + g, "C")]
                        for cbk, kind in blocks:
                            P.dma(P.sp, xin[:, 3:3 + S], self.scr["s_xbcT"][cbk * 128:(cbk + 1) * 128, :], reads=[self.scrb["s_xbcT"]], writes=[xin], sembuf=xin)
                            P.v(lambda: nc.vector.tensor_scalar(acc[:], xin[:, 3:3 + S], convw[:, cbk * 4 + 3:cbk * 4 + 4], convb[:, cbk:cbk + 1], ALU.mult, ALU.add),
                                [xin, ppt], [acc])
                            for k in range(3):
                                P.v(lambda: nc.vector.scalar_tensor_tensor(acc[:], xin[:, k:k + S], convw[:, cbk * 4 + k:cbk * 4 + k + 1], acc[:], ALU.mult, ALU.add),
                                    [xin, ppt, acc], [acc])
                            dstt = {"x0": xcb, "x1": xcb, "B": BT, "C": CT}[kind]
                            P.actf(dstt[:], acc[:], AF.Silu, [acc], [dstt])
                            if kind != "C":
                                for n4 in range(8):
                                    ps = pool.next()
                                    psb = ps[:].bitcast(BF16)
                                    for j in range(4):
                                        n = n4 * 4 + j
                                        P.op(P.pe, lambda: nc.tensor.transpose(psb[:, j * 128:(j + 1) * 128], dstt[:, n * 128:(n + 1) * 128], self.cbc("ident")),
                                             [dstt, self.cb], [ps], signal=(j == 3))
                                    srcv = psb[:, 0:512].rearrange("p (a b) -> p a b", a=4)
                                    if kind == "B":
                                        dv_, db_ = Btm[:, n4 * 4:(n4 + 1) * 4, :], Btm
                                    else:
                                        co = 0 if kind == "x0" else 128
                                        dv_, db_ = xtm[:, n4 * 4:(n4 + 1) * 4, co:co + 128], xtm
                                    P.v(lambda: nc.vector.tensor_copy(dv_, srcv), [ps], [db_])
                    if DBG.get('ssd_stop') == 2:
                        continue
                    with Scope(P) as s2:
                        xd = s2.tile("xd", [128, NT, 256], BF16)
                        xs = s2.tile("xs", [128, NT, 256], BF16)
                        hprev = s2.tile("hprev", [128, NT, 256], BF16)
                        Sg = s2.tile("Sg", [128, 256], F32)
                        tmpS = s2.tile("tmpS", [128, 256], F32)
                        t1 = Rot([s2.tile("t1", [128, 512], F32) for _ in range(2)])
                        dec = Rot([s2.tile("dec", [128, 512], F32) for _ in range(2)])
                        dfs = Rot([s2.tile("dfs", [128, 512], F32) for _ in range(2)])
                        MT = Rot([s2.tile("MT", [128, 4, 128], BF16) for _ in range(2)])
                        CTh = Rot([s2.tile("CTh", [128, 4, 128], BF16) for _ in range(2)])
                        yz = [s2.tile("yz", [64, 512], F32) for _ in range(4)]
                        sqs = [s2.tile("sqs", [64, 512], F32) for _ in range(4)]
                        lnv = s2.tile("lnv", [64, 512], F32)
                        rs = s2.tile("rs", [64, 512], F32)
                        zb = Rot([s2.tile("zb", [64, 512], BF16) for _ in range(4)])
                        ob = Rot([s2.tile("ob", [64, 512], BF16) for _ in range(4)])
                        accp = Rot(self.ps[0:4])
                        scp = Rot(self.ps[4:8])
                        h4 = slice(4 * g, 4 * g + 4)
                        xv = xtm[:].rearrange("p n (h c) -> p n h c", h=4)
                        P.v(lambda: nc.vector.tensor_tensor(xd[:].rearrange("p n (h c) -> p n h c", h=4), xv,
                                                            dtv[:, :, h4].unsqueeze(3).broadcast_to([128, NT, 4, 64]), ALU.mult), [xtm, dtv], [xd])
                        P.v(lambda: nc.vector.tensor_tensor(xs[:].rearrange("p n (h c) -> p n h c", h=4), xv,
                                                            wts[:, :, h4].unsqueeze(3).broadcast_to([128, NT, 4, 64]), ALU.mult), [xtm, wts], [xs])
                        P.v(lambda: nc.vector.memset(Sg[:], 0.0), [], [Sg])
                        for n in range(NT):
                            ps = scp.next()
                            P.mm(ps[:, 0:256], Btm[:, n, :], xs[:, n, :], True, True, [Btm, xs], [ps])
                            P.v(lambda: nc.vector.tensor_copy(hprev[:, n, :], Sg[:]), [Sg], [hprev])
                            P.v(lambda: nc.vector.tensor_tensor(tmpS[:].rearrange("p (h c) -> p h c", h=4), Sg[:].rearrange("p (h c) -> p h c", h=4),
                                                                cdec[:, n, h4].unsqueeze(2).broadcast_to([128, 4, 64]), ALU.mult), [Sg, cdec], [tmpS])
                            P.v(lambda: nc.vector.tensor_tensor(Sg[:], tmpS[:], ps[:, 0:256], ALU.add), [tmpS, ps], [Sg])
                        gn = self.ppc(ppt, "ssdn")
                        steps = []
                        for n4 in range(0 if DBG.get('ssd_stop') == 3 else 8):
                            grp = {}
                            for j in range(4):
                                steps.append(dict(n4=n4, j=j, n=n4 * 4 + j, grp=grp))

                        def front(st):
                            n = st["n"]
                            psa = scp.next()
                            for hh in range(4):
                                P.mm(psa[:, hh * 128:(hh + 1) * 128], a[:, n, 4 * g + hh:4 * g + hh + 1].broadcast_to([128, 128]), self.cfc("tri"),
                                     True, True, [a, self.cf], [psa], signal=(hh == 3))
                            pscb = scp.next()
                            P.mm(pscb[:, 0:128], BT[:, n * 128:(n + 1) * 128], CT[:, n * 128:(n + 1) * 128], True, True, [BT, CT], [pscb])
                            t1t = t1.next()
                            P.v(lambda: nc.vector.tensor_tensor(t1t[:], psa[:], self.cfc("negmask4"), ALU.add), [psa, self.cf], [t1t])
                            P.v(lambda: nc.vector.tensor_tensor(t1t[:].rearrange("p (h c) -> p h c", h=4), t1t[:].rearrange("p (h c) -> p h c", h=4),
                                                                acum[:, n, h4].unsqueeze(2).broadcast_to([128, 4, 128]), ALU.subtract), [t1t, acum], [t1t])
                            dect = dec.next()
                            P.actf(dect[:], t1t[:], AF.Exp, [t1t], [dect])
                            mt = MT.next()
                            P.v(lambda: nc.vector.tensor_tensor(mt[:], dect[:].rearrange("p (h c) -> p h c", h=4),
                                                                pscb[:, 0:128].unsqueeze(1).broadcast_to([128, 4, 128]), ALU.mult), [dect, pscb], [mt])
                            dfst = dfs.next()
                            P.actf(dfst[:], psa[:], AF.Exp, [psa], [dfst])
                            cth = CTh.next()
                            P.v(lambda: nc.vector.tensor_tensor(cth[:], dfst[:].rearrange("p (h c) -> p h c", h=4),
                                                                CT[:, n * 128:(n + 1) * 128].unsqueeze(1).broadcast_to([128, 4, 128]), ALU.mult), [dfst, CT], [cth])
                            st["mt"] = mt
                            st["cth"] = cth

                        def back(st):
                            n4, j, n, grp = st["n4"], st["j"], st["n"], st["grp"]
                            if j == 0:
                                grp["y"] = [accp.next() for _ in range(4)]
                            ybank = grp["y"]
                            mt, cth = st["mt"], st["cth"]
                            for hh in range(4):
                                yo = ybank[hh][0:64, j * 128:(j + 1) * 128]
                                P.mm(yo, xd[:, n, hh * 64:(hh + 1) * 64], mt[:, hh, :], True, False, [xd, mt], [ybank[hh]], signal=False)
                                P.mm(yo, hprev[:, n, hh * 64:(hh + 1) * 64], cth[:, hh, :], False, False, [hprev, cth], [ybank[hh]], signal=False)
                                P.mm(yo, xtm[:, n, hh * 64:(hh + 1) * 64], DI[:, 4 * g + hh, :], False, True, [xtm, DI], [ybank[hh]], signal=True)
                            if j == 3:
                                srcs = []
                                for hh in range(4):
                                    hd = 4 * g + hh
                                    zt = zb.next()
                                    P.dma(P.sp, zt[:], self.scr["s_zT"][hd * 64:(hd + 1) * 64, n4 * 512:(n4 + 1) * 512], reads=[self.scrb["s_zT"]], writes=[zt], sembuf=zt)
                                    P.v(lambda: nc.vector.tensor_tensor(yz[hh][:], ybank[hh][0:64, :], zt[:], ALU.mult), [ybank[hh], zt], [yz[hh]])
                                    srcs.append((yz[hh][:], yz[hh]))
                                rsx = self.rms_feat(srcs, 64, "mean256", None, None, None, scp, (sqs, lnv, rs))
                                for hh in range(4):
                                    hd = 4 * g + hh
                                    o = ob.next()
                                    P.v(lambda: nc.vector.scalar_tensor_tensor(o[:], yz[hh][:], gn[0:64, hd:hd + 1], rsx[0:64, :], ALU.mult, ALU.mult),
                                        [yz[hh], ppt, rs], [o])
                                    P.dma(P.sp, self.scr["mixT"][512 + hd * 64:512 + (hd + 1) * 64, n4 * 512:(n4 + 1) * 512], o[:],
                                          reads=[o], writes=[self.scrb["mixT"]], sembuf=o)

                        self.pipeline(steps, front, back, 1)


_CACHE = {}


def _get_prog(key, **kw):
    if key not in _CACHE:
        _CACHE[key] = K(**kw)
    return _CACHE[key]


def make_in_maps(inputs, ncores):
    pp = _pack_params(inputs)
    maps = []
    for b in range(ncores):
        m = {
            "x": np.ascontiguousarray(inputs["x"][b]),
            "w_in": inputs["w_in"], "w_out": inputs["w_out"], "w_gate": inputs["w_gate"],
            "w_up": inputs["w_up"], "w_down": inputs["w_down"],
            "norm_mix_pre": inputs["norm_mix_pre"], "norm_mix_post": inputs["norm_mix_post"],
            "norm_ffn_pre": inputs["norm_ffn_pre"], "norm_ffn_post": inputs["norm_ffn_post"],
            "pp": pp, "cfa": CFA, "cba": CBA, "aug": AUG,
        }
        maps.append(m)
    return maps


def kernel(**inputs):
    inputs = {k: np.asarray(v) for k, v in inputs.items()}
    _pack_params(inputs)
    prog = _get_prog("full", layers=list(range(NL)))
    maps = make_in_maps(inputs, 8)
    res = run_bass_kernel_spmd(prog.nc, maps, core_ids=list(range(8)))
    return np.stack([np.asarray(r["out"], dtype=np.float32) for r in res.results], 0)
```

```python
import math
from contextlib import ExitStack
import numpy as np
import ml_dtypes
import concourse.bass as bass
import concourse.mybir as mybir
from concourse.bass_utils import run_bass_kernel_spmd

F32 = mybir.dt.float32
BF16 = mybir.dt.bfloat16
AF = mybir.ActivationFunctionType
ALU = mybir.AluOpType
AX = mybir.AxisListType

S = 4096
D = 2048
NL = 4
INC = 6152
HID = 5632
EPS = 1e-6
NEG = -262144.0
NT = S // 128
TP = 512
NPASS = S // TP
DBG = {}


def DS(start, count, step=1):
    return slice(start, start + (count - 1) * step + 1, step)


class Sem:
    def __init__(self, h):
        self.h = h
        self.total = 0


class Buf:
    def __init__(self, name):
        self.name = name
        self.writes = {}
        self.reads = {}
        self.dsem = None
        self.psum = False


class Tile:
    def __init__(self, t, buf):
        self.t = t
        self.buf = buf

    def __getitem__(self, k):
        return self.t[k]


class Eng:
    def __init__(self, h, sem, kind):
        self.h = h
        self.sem = sem
        self.known = {}
        self.kind = kind


def _b(x):
    return x.buf if isinstance(x, Tile) else x


class Prog:
    def __init__(self, nc, es):
        self.nc = nc
        self.es = es
        self.allsems = []
        self.free_dsems = []

        def mk(h, kind):
            s = self.newsem()
            return Eng(h, s, kind)

        self.pe = mk(nc.tensor, "pe")
        self.act = mk(nc.scalar, "act")
        self.dve = mk(nc.vector, "dve")
        self.pool = mk(nc.gpsimd, "pool")
        self.sp = mk(nc.sync, "sp")
        self.engs = [self.pe, self.act, self.dve, self.pool, self.sp]
        self.castsems = [self.newsem() for _ in range(8)]
        self.castk = 0
        self.nid = 0
        self.dq = []

    def newsem(self):
        h = self.es.enter_context(self.nc.semaphore(f"s{len(self.allsems)}"))
        s = Sem(h)
        self.allsems.append(s)
        return s

    def dsem(self, buf):
        if buf.dsem is None:
            buf.dsem = self.free_dsems.pop() if self.free_dsems else self.newsem()
        return buf.dsem

    def _wait(self, eng, deps):
        for sem, val in deps.items():
            if val > eng.known.get(sem, 0):
                eng.h.wait_ge(sem.h, val)
                eng.known[sem] = val

    def op(self, eng, fn, reads=(), writes=(), signal=True):
        reads = [_b(x) for x in reads]
        writes = [_b(x) for x in writes]
        deps = {}

        def add(sem, val, raw):
            if sem is eng.sem:
                if not raw or eng.kind == "pe":
                    return
            if val > deps.get(sem, 0):
                deps[sem] = val

        for b in reads:
            for sem, val in b.writes.items():
                add(sem, val, True)
            if b.psum:
                for sem, val in b.reads.items():
                    add(sem, val, False)
        for b in writes:
            for sem, val in b.writes.items():
                add(sem, val, False)
            for sem, val in b.reads.items():
                add(sem, val, False)
        self._wait(eng, deps)
        ins = fn()
        cnt = eng.sem.total + 1
        if signal:
            ins.then_inc(eng.sem.h, 1)
            eng.sem.total = cnt
        for b in reads:
            if cnt > b.reads.get(eng.sem, 0):
                b.reads[eng.sem] = cnt
        for b in writes:
            if cnt > b.writes.get(eng.sem, 0):
                b.writes[eng.sem] = cnt
        return ins

    def dma(self, q, out, in_, reads=(), writes=(), sembuf=None, sem=None):
        reads = [_b(x) for x in reads]
        writes = [_b(x) for x in writes]
        if sem is None:
            sem = self.dsem(_b(sembuf))
        deps = {}
        for b in reads:
            for s_, v in b.writes.items():
                deps[s_] = max(deps.get(s_, 0), v)
        for b in writes:
            for s_, v in b.reads.items():
                deps[s_] = max(deps.get(s_, 0), v)
            for s_, v in b.writes.items():
                if s_ is not sem:
                    deps[s_] = max(deps.get(s_, 0), v)
        self._wait(q, deps)
        q.h.dma_start(out=out, in_=in_).then_inc(sem.h, 16)
        sem.total += 16
        for b in reads:
            b.reads[sem] = sem.total
        for b in writes:
            b.writes[sem] = sem.total

    def dstore(self, *a, **kw):
        self.dq.append((a, kw))

    def dflush_for(self, tile, keep=1):
        b = _b(tile)
        if any(b in [_b(x) for x in kw.get("reads", ())] for a, kw in self.dq):
            self.dflush(0)
        else:
            self.dflush(keep)

    def dflush(self, keep=0):
        while len(self.dq) > keep:
            a, kw = self.dq.pop(0)
            self.dma(*a, **kw)

    def barrier(self):
        self.dflush(0)
        for e in self.engs:
            for s in self.allsems:
                if s.total > e.known.get(s, 0):
                    e.h.wait_ge(s.h, s.total)
                    e.known[s] = s.total

    def mm(self, out, lhsT, rhs, start, stop, reads, writes, signal=True):
        nc = self.nc
        return self.op(self.pe, lambda: nc.tensor.matmul(out, lhsT, rhs, start=start, stop=stop),
                       reads, writes, signal)

    def actf(self, out, in_, func, reads, writes, bias=None, scale=1.0, accum_out=None):
        nc = self.nc
        kw = {}
        if bias is not None:
            kw["bias"] = bias
        if accum_out is not None:
            kw["accum_out"] = accum_out
        return self.op(self.act, lambda: nc.scalar.activation(out=out, in_=in_, func=func, scale=scale, **kw),
                       reads, writes)

    def v(self, fn, reads, writes):
        return self.op(self.dve, fn, reads, writes)


class Scope:
    def __init__(self, P):
        self.P = P
        self.es = ExitStack()
        self.tiles = []

    def __enter__(self):
        self.es.__enter__()
        return self

    def tile(self, name, shape, dt):
        P = self.P
        P.nid += 1
        t = self.es.enter_context(P.nc.sbuf_tensor(f"{name}_{P.nid}", list(shape), dt))
        T = Tile(t, Buf(name))
        self.tiles.append(T)
        return T

    def __exit__(self, *a):
        self.P.barrier()
        for T in self.tiles:
            if T.buf.dsem is not None:
                self.P.free_dsems.append(T.buf.dsem)
                T.buf.dsem = None
        return self.es.__exit__(*a)


class Pref:
    def __init__(self, n, bufs, loadfn, dist):
        self.n, self.bufs, self.loadfn, self.dist, self.issued = n, bufs, loadfn, dist, 0

    def get(self, i):
        while self.issued < min(self.n, i + self.dist + 1):
            j = self.issued
            self.loadfn(j, self.bufs[j % len(self.bufs)])
            self.issued += 1
        return self.bufs[i % len(self.bufs)]


class Rot:
    def __init__(self, items):
        self.items = items
        self.i = 0

    def next(self):
        x = self.items[self.i % len(self.items)]
        self.i += 1
        return x


CF = {}
CB = {}


def _build_consts():
    cf = []
    cb = []

    def addf(name, arr):
        arr = np.asarray(arr, np.float32)
        assert arr.shape[0] == 128
        c0 = sum(a.shape[1] for a in cf)
        CF[name] = (c0, arr.shape[1])
        cf.append(arr)

    def addb(name, arr):
        arr = np.asarray(arr, np.float32)
        assert arr.shape[0] == 128
        c0 = sum(a.shape[1] for a in cb)
        CB[name] = (c0, arr.shape[1])
        cb.append(arr)

    idx = np.arange(128)
    addf("eps", np.full((128, 1), EPS))
    addf("one", np.ones((128, 1)))
    addf("tri", (idx[:, None] <= idx[None, :]).astype(np.float32))
    addf("onesf", np.ones((128, 128)))
    addf("mean128", np.full((128, 128), 1.0 / 128))
    addf("mean256", np.full((128, 64), 1.0 / 256))
    nm = np.where(idx[None, :] >= idx[:, None], 0.0, -30000.0)
    addf("negmask4", np.tile(nm, (1, 4)))
    for h in range(4):
        lg = math.log1p(-2.0 ** (-5.0 - h))
        rel = idx[None, :] - idx[:, None]
        rm = np.where(rel >= 0, np.exp(lg * np.maximum(rel, 0)), 0.0) * (64 ** -0.5)
        addf(f"rmask{h}", np.tile(rm, (1, 4)))
        addf(f"xi{h}", np.tile(np.exp(lg * (idx + 1.0))[None, :], (128, 1)))
        addf(f"zeta{h}", (np.exp(lg * (127.0 - idx)) * (64 ** -0.5))[:, None])
    addb("ident", np.eye(128))
    addb("ones", np.ones((128, 128)))
    md = np.zeros((128, 512))
    md[:, :128] = np.where(idx[:, None] > idx[None, :], NEG, 0.0)
    addb("mdiag", md)
    ii = np.arange(256)
    band = np.where((idx[:, None] <= ii[None, :]) & (ii[None, :] <= idx[:, None] + 128), 0.0, NEG)
    addb("band", band)
    cfa = np.concatenate(cf, 1).astype(np.float32)
    cba = np.concatenate(cb, 1).astype(ml_dtypes.bfloat16)
    t = np.arange(S)
    aug = np.zeros((12, 2, 4, S), np.float32)
    slopes = [2.0 ** (-8.0 * (h + 1) / 4) for h in range(4)] + [2.0 ** (-8.0 * (h + 1) / 8) for h in range(8)]
    for a, s in enumerate(slopes):
        aug[a, 0, 0] = 8 * s * (t % 128)
        aug[a, 0, 1] = 8 * s * 128 * (t // 128)
        aug[a, 0, 2] = 1
        aug[a, 0, 3] = 1
        aug[a, 1, 0] = 1
        aug[a, 1, 1] = 1
        aug[a, 1, 2] = -8 * s * (t % 128)
        aug[a, 1, 3] = -8 * s * 128 * (t // 128)
    return cfa, cba, aug.astype(ml_dtypes.bfloat16)


CFA, CBA, AUG = _build_consts()

PPC = {}


def _pack_params(inp):
    cols = []

    def add(name, arr):
        c0 = sum(a.shape[2] for a in cols)
        PPC[name] = (c0, arr.shape[2])
        cols.append(np.ascontiguousarray(arr, dtype=np.float32))

    add("retn", inp["ret_norm"].reshape(NL, 4, 128).transpose(0, 2, 1))
    add("convw", inp["ssd_conv_w"].reshape(NL, 8, 128, 4).transpose(0, 2, 1, 3).reshape(NL, 128, 32))
    add("convb", inp["ssd_conv_b"].reshape(NL, 8, 128).transpose(0, 2, 1))
    add("dtb", np.broadcast_to(inp["ssd_dt_bias"][:, None, :], (NL, 128, 8)))
    add("alog", np.broadcast_to(inp["ssd_a_log"][:, None, :], (NL, 128, 8)))
    add("dsk", np.broadcast_to(inp["ssd_d"][:, None, :], (NL, 128, 8)))
    sn = inp["ssd_norm"].reshape(NL, 8, 64).transpose(0, 2, 1)
    add("ssdn", np.concatenate([sn, sn], 1))
    add("lam", np.broadcast_to(inp["diff_lambda"].reshape(NL, 1, 256), (NL, 128, 256)))
    add("difn", inp["diff_norm"].reshape(NL, 128, 1))
    return np.concatenate(cols, 2)


A_BLOCKS = [
    (0, 512, "F", "r_qkT", 0, "copy"),
    (256, 256, "T", "r_ktm", 0, "copy"),
    (512, 512, "T", "r_vtm", 0, "copy"),
    (1024, 512, "F", "r_gT", 0, "silu"),
    (1536, 512, "F", "s_zT", 0, "silu"),
    (2048, 512, "F", "s_xbcT", 0, "copy"),
    (2560, 512, "F", "s_xbcT", 512, "copy"),
    (3072, 8, "T", "s_dt", 0, "copy32"),
    (3080, 512, "F", "d_qT", 0, "copy"),
    (3592, 512, "F", "d_kT", 0, "copy"),
    (4104, 512, "T", "d_vtm", 0, "copy"),
    (4616, 512, "F", "l_qT", 0, "copy"),
    (5128, 512, "F", "l_kT", 0, "copy"),
    (5640, 512, "T", "l_vtm", 0, "copy"),
]
SCR = {
    "r_qkT": ([512, S], BF16), "r_ktm": ([S, 256], BF16), "r_vtm": ([S, 512], BF16),
    "r_gT": ([512, S], BF16), "s_zT": ([512, S], BF16), "s_xbcT": ([1024, S], BF16),
    "s_dt": ([S, 8], F32), "d_qT": ([512, S], BF16), "d_kT": ([512, S], BF16),
    "d_vtm": ([S, 512], BF16), "l_qT": ([512, S], BF16), "l_kT": ([512, S], BF16),
    "l_vtm": ([S, 512], BF16), "mixT": ([D, S], BF16),
}


class K:
    def __init__(self, layers, debug=False, phases="ABCD", mixers="abcd"):
        self.layers = layers
        self.debug = debug
        self.phases = phases
        self.mixers = mixers
        self.nc = bass.Bass("TRN2", target_bir_lowering=False)
        self.build()

    def dram_in(self, name, shape, dt):
        return self.nc.dram_tensor(name, list(shape), dt, kind="ExternalInput").ap()

    def build(self):
        nc = self.nc
        self.x_in = self.dram_in("x", [S, D], F32)
        if not DBG.get("now"):
            self.w_in = self.dram_in("w_in", [NL, D, INC], F32)
            self.w_out = self.dram_in("w_out", [NL, D, D], F32)
            self.w_gate = self.dram_in("w_gate", [NL, D, HID], F32)
            self.w_up = self.dram_in("w_up", [NL, D, HID], F32)
            self.w_down = self.dram_in("w_down", [NL, HID, D], F32)
        self.gn = [self.dram_in(n, [NL, D], F32) for n in ("norm_mix_pre", "norm_mix_post", "norm_ffn_pre", "norm_ffn_post")]
        self.pp = self.dram_in("pp", [NL, 128, sum(v[1] for v in PPC.values())], F32)
        self.cfa = self.dram_in("cfa", list(CFA.shape), F32)
        self.cba = self.dram_in("cba", list(CBA.shape), BF16)
        self.aug = self.dram_in("aug", list(AUG.shape), BF16)
        self.out = nc.dram_tensor("out", [S, D], F32, kind="ExternalOutput").ap()
        if DBG.get('dbgw'):
            self.dbgw = nc.dram_tensor("dbgw", [128, 4096], BF16, kind="ExternalOutput").ap()
            self.dbgwb = Buf('dbgw')
        kind = "ExternalOutput" if self.debug else "Internal"
        self.scr = {}
        self.scrb = {}
        for n, (shp, dt) in SCR.items():
            if DBG.get("now"):
                kind = "ExternalOutput" if n == "mixT" else "ExternalInput"
            self.scr[n] = nc.dram_tensor("scr_" + n, shp, dt, kind=kind).ap()
            self.scrb[n] = Buf("scr_" + n)
        self.outb = Buf("out")
        self.outbs = [Buf(f"out{i}") for i in range(NT)]
        self.wq = {}
        self.wqb = {}
        for l in ([] if DBG.get("now") else self.layers):
            tot = sum(b[1] for b in A_BLOCKS)
            self.wq[("in", l)] = nc.dram_tensor(f"wq_in{l}", [128, 16 * tot], BF16, kind="Internal").ap()
            self.wq[("out", l)] = nc.dram_tensor(f"wq_out{l}", [128, 16 * D], BF16, kind="Internal").ap()
            self.wq[("gate", l)] = nc.dram_tensor(f"wq_gate{l}", [128, 16 * HID], BF16, kind=("ExternalOutput" if DBG.get("dbgw") else "Internal")).ap()
            self.wq[("up", l)] = nc.dram_tensor(f"wq_up{l}", [128, 16 * HID], BF16, kind="Internal").ap()
            self.wq[("down", l)] = nc.dram_tensor(f"wq_down{l}", [128, 44 * D], BF16, kind="Internal").ap()
        with ExitStack() as es:
            self.P = P = Prog(nc, es)
            with Scope(P) as g:
                self.g = g
                self.cf = g.tile("cf", list(CFA.shape), F32)
                self.cb = g.tile("cb", list(CBA.shape), BF16)
                P.dma(P.sp, self.cf[:], self.cfa[:], writes=[self.cf], sembuf=self.cf)
                P.dma(P.sp, self.cb[:], self.cba[:], writes=[self.cb], sembuf=self.cb)
                self.ps = []
                for i in range(8):
                    t = es.enter_context(nc.psum_tensor(f"psb{i}", [128, 512], F32))
                    self.ps.append(Tile(t, Buf(f"ps{i}")))
                    self.ps[-1].buf.psum = True
                self.cast_plan = []
                self.plan_casts()
                self.emit_casts(len(A_BLOCKS))
                first = True
                for l in self.layers:
                    xsrc, xsb = (self.x_in, None) if first else (self.out, self.outb)
                    if "A" in self.phases:
                        self.phase_A(l, xsrc, xsb)
                    if "B" in self.phases:
                        self.phase_B(l)
                    if "C" in self.phases:
                        self.phase_C(l, xsrc, xsb)
                    if "D" in self.phases:
                        self.phase_D(l)
                    first = False
                self.emit_casts(10 ** 9)
            P.barrier()

    def cfc(self, name, rows=128):
        c0, n = CF[name]
        return self.cf[0:rows, c0:c0 + n]

    def cbc(self, name, rows=128):
        c0, n = CB[name]
        return self.cb[0:rows, c0:c0 + n]

    def ppc(self, T, name):
        c0, n = PPC[name]
        return T[:, c0:c0 + n]

    def plan_casts(self):
        for l in ([] if DBG.get("now") else self.layers):
            off = 0
            for bi, (c0, n, *_r) in enumerate(A_BLOCKS):
                src = self.w_in[l, :, c0:c0 + n].rearrange("(k p) c -> p k c", p=128)
                dst = self.wq[("in", l)][:, off:off + 16 * n].rearrange("p (k c) -> p k c", k=16)
                b = Buf(f"wqin{l}_{bi}")
                self.wqb[("in", l, bi)] = (dst, b)
                b.cast_idx = len(self.cast_plan)
                self.cast_plan.append((src, dst, b))
                off += 16 * n
            for cbk in range(4):
                src = self.w_out[l, :, cbk * 512:(cbk + 1) * 512].rearrange("(k p) c -> p k c", p=128)
                dst = self.wq[("out", l)][:, cbk * 8192:(cbk + 1) * 8192].rearrange("p (k c) -> p k c", k=16)
                b = Buf("wqo")
                self.wqb[("out", l, cbk)] = (dst, b)
                b.cast_idx = len(self.cast_plan)
                self.cast_plan.append((src, dst, b))
            for nm, W in (("gate", self.w_gate), ("up", self.w_up)):
                for cbk in range(22):
                    src = W[l, :, cbk * 256:(cbk + 1) * 256].rearrange("(k p) c -> p k c", p=128)
                    dst = self.wq[(nm, l)][:, cbk * 4096:(cbk + 1) * 4096].rearrange("p (k c) -> p k c", k=16)
                    b = Buf("wqg")
                    self.wqb[(nm, l, cbk)] = (dst, b)
                    b.cast_idx = len(self.cast_plan)
                    self.cast_plan.append((src, dst, b))
            for cbk in range(4):
                for kg in range(4):
                    src = self.w_down[l, kg * 1408:(kg + 1) * 1408, cbk * 512:(cbk + 1) * 512].rearrange("(k p) c -> p k c", p=128)
                    o = (cbk * 4 + kg) * 11 * 512
                    dst = self.wq[("down", l)][:, o:o + 11 * 512].rearrange("p (k c) -> p k c", k=11)
                    b = Buf("wqd")
                    self.wqb[("down", l, cbk, kg)] = (dst, b)
                    b.cast_idx = len(self.cast_plan)
                    self.cast_plan.append((src, dst, b))
        self.cast_i = 0

    def ensure_cast(self, b):
        if self.cast_i <= b.cast_idx:
            self.emit_casts(b.cast_idx - self.cast_i + 1)

    def emit_casts(self, n):
        P = self.P
        while n > 0 and self.cast_i < len(self.cast_plan):
            src, dst, b = self.cast_plan[self.cast_i]
            sem = P.castsems[P.castk % len(P.castsems)]
            P.castk += 1
            if sem.total > P.pool.known.get(sem, 0):
                P.pool.h.wait_ge(sem.h, sem.total)
                P.pool.known[sem] = sem.total
            P.dma(P.pool, dst, src, writes=[b], sem=sem)
            self.cast_i += 1
            n -= 1

    def rstd_from_ssq(self, ssq, lnv, rstd, scale):
        P = self.P
        P.actf(lnv[:], ssq[:], AF.Ln, [ssq, self.cf], [lnv], bias=self.cfc("eps"), scale=scale)
        P.actf(rstd[:], lnv[:], AF.Exp, [lnv], [rstd], scale=-0.5)

    def norm_part(self, X, Hb, gT, small):
        P, nc = self.P, self.nc
        ssq, lnv, rstd = small
        P.actf(Hb[:], X[:], AF.Square, [X], [Hb, ssq], accum_out=ssq[:])
        self.rstd_from_ssq(ssq, lnv, rstd, 1.0 / D)
        P.v(lambda: nc.vector.scalar_tensor_tensor(Hb[:], X[:], rstd[:], gT[:], ALU.mult, ALU.mult),
            [X, rstd, gT], [Hb])

    def trans_part(self, Hb, HT, tt, psrot, ei):
        P, nc = self.P, self.nc
        ident = self.cbc("ident")
        for k4 in range(4):
            ps = psrot.next()
            psb = ps[:].bitcast(BF16)
            for kk in range(4):
                k = k4 * 4 + kk
                P.op(P.pe, lambda: nc.tensor.transpose(psb[:, kk * 128:(kk + 1) * 128], Hb[:, k * 128:(k + 1) * 128], ident),
                     [Hb, self.cb], [ps], signal=(kk == 3))
            dst = HT[:, k4 * 4:(k4 + 1) * 4, tt * 128:(tt + 1) * 128]
            src = psb[:, 0:512].rearrange("p (a b) -> p a b", a=4)
            if (ei + k4) % 2 == 0:
                P.op(P.act, lambda: nc.scalar.copy(out=dst, in_=src), [ps], [HT])
            else:
                P.v(lambda: nc.vector.tensor_copy(dst, src), [ps], [HT])

    def norm_transpose(self, X, Hb, gT, HT, tt, small, psrot, ei):
        self.norm_part(X, Hb, gT, small)
        self.trans_part(Hb, HT, tt, psrot, ei)

    def load_g(self, T, which, l):
        P = self.P
        P.dma(P.sp, T[:], self.gn[which][l:l + 1, :].broadcast_to([128, D]), writes=[T], sembuf=T)

    def post_norm_residual(self, Y, X, gT, small, dst_rows):
        P, nc = self.P, self.nc
        ssq, lnv, rstd = small
        P.actf(self.junk[:], Y[:], AF.Square, [Y], [self.junk, ssq], accum_out=ssq[:])
        self.rstd_from_ssq(ssq, lnv, rstd, 1.0 / D)
        P.v(lambda: nc.vector.scalar_tensor_tensor(Y[:], Y[:], rstd[:], gT[:], ALU.mult, ALU.mult), [Y, rstd, gT], [Y])
        P.v(lambda: nc.vector.tensor_tensor(X[:], X[:], Y[:], ALU.add), [X, Y], [X])
        P.dstore(P.act, self.out[dst_rows, :], X[:], reads=[X], writes=[self.outbs[dst_rows.start // 128]], sembuf=X)

    def phase_A(self, l, xsrc, xsb):
        P, nc = self.P, self.nc
        TA = 1024
        NPA = S // TA
        NTT = TA // 128
        with Scope(P) as sc:
            xt = Rot([sc.tile("xt", [128, D], F32) for _ in range(2)])
            hbs = [sc.tile("hb", [128, D], BF16) for _ in range(4)]
            hT = [sc.tile("hT", [128, 16, TA], BF16) for _ in range(2)]
            wbufs = [sc.tile("wb", [128, 16, 512], BF16) for _ in range(3)]
            stg = Rot([sc.tile("stg", [128, 512], BF16) for _ in range(4)])
            stg32 = Rot([sc.tile("stg32", [128, 8], F32) for _ in range(2)])
            g1 = sc.tile("g1", [128, D], F32)
            smalls = Rot([[sc.tile("sm", [128, 1], F32) for _ in range(3)] for _ in range(2)])
            psrot = Rot(self.ps)
            self.load_g(g1, 0, l)
            nb = len(A_BLOCKS)

            def loadw(j, W):
                bi = j % nb
                n = A_BLOCKS[bi][1]
                wsrc, wbuf = self.wqb[("in", l, bi)]
                self.ensure_cast(wbuf)
                P.dma(P.sp, W[:, :, 0:n], wsrc, reads=[wbuf], writes=[W], sembuf=W)

            wp = Pref(NPA * nb, wbufs, loadw, 2)
            self.ei = 0

            def do_norm_a(tp, half):
                for t4 in range(4):
                    tt = half * 4 + t4
                    t0 = tp * TA + tt * 128
                    X = xt.next()
                    P.dma(P.sp, X[:], xsrc[t0:t0 + 128, :], reads=[self.outbs[t0 // 128]] if xsb else [], writes=[X], sembuf=X)
                    self.norm_part(X, hbs[t4], g1, smalls.next())

            def do_norm_b(tp, half):
                for t4 in range(4):
                    self.trans_part(hbs[t4], hT[tp % 2], half * 4 + t4, psrot, self.ei)
                    self.ei += 1

            for half in range(2):
                do_norm_a(0, half)
                do_norm_b(0, half)
            for tp in range(NPA):
                self.emit_casts(24 if l == self.layers[0] else 0)
                HT = hT[tp % 2]
                for bi, (c0, n, orient, dname, doff, func) in enumerate(A_BLOCKS):
                    W = wp.get(tp * nb + bi)
                    if tp + 1 < NPA:
                        if bi == 4:
                            do_norm_a(tp + 1, 0)
                        elif bi == 6:
                            do_norm_b(tp + 1, 0)
                        elif bi == 8:
                            do_norm_a(tp + 1, 1)
                        elif bi == 10:
                            do_norm_b(tp + 1, 1)
                    dst = self.scr[dname]
                    dbuf = self.scrb[dname]
                    if orient == "F":
                        for cc in range(n // 128):
                            pss = [psrot.next() for _ in range(TA // 512)]
                            for k in range(16):
                                for tg, ps in enumerate(pss):
                                    P.mm(ps[:, :], W[:, k, cc * 128:(cc + 1) * 128], HT[:, k, tg * 512:(tg + 1) * 512], k == 0, k == 15,
                                         [W, HT], [ps], signal=(k == 15))
                            for tg, ps in enumerate(pss):
                                st = stg.next()
                                P.dflush(2)
                                if func == "silu":
                                    P.actf(st[:], ps[:], AF.Silu, [ps], [st])
                                elif self.ei % 2 == 0:
                                    P.op(P.act, lambda: nc.scalar.copy(out=st[:], in_=ps[:]), [ps], [st])
                                else:
                                    P.v(lambda: nc.vector.tensor_copy(st[:], ps[:]), [ps], [st])
                                self.ei += 1
                                r0 = doff + cc * 128
                                c_0 = tp * TA + tg * 512
                                P.dstore(P.act, dst[r0:r0 + 128, c_0:c_0 + 512], st[:], reads=[st], writes=[dbuf], sembuf=st)
                    else:
                        for tt in range(NTT):
                            ps = psrot.next()
                            for k in range(16):
                                P.mm(ps[:, 0:n], HT[:, k, tt * 128:(tt + 1) * 128], W[:, k, 0:n], k == 0, k == 15,
                                     [W, HT], [ps], signal=(k == 15))
                            st = stg32.next() if func == "copy32" else stg.next()
                            P.dflush(1 if func == "copy32" else 2)
                            if self.ei % 2 == 0:
                                P.op(P.act, lambda: nc.scalar.copy(out=st[:, 0:n], in_=ps[:, 0:n]), [ps], [st])
                            else:
                                P.v(lambda: nc.vector.tensor_copy(st[:, 0:n], ps[:, 0:n]), [ps], [st])
                            self.ei += 1
                            t0 = tp * TA + tt * 128
                            P.dstore(P.act, dst[t0:t0 + 128, doff:doff + n], st[:, 0:n], reads=[st], writes=[dbuf], sembuf=st)
            P.dflush(0)

    def phase_C(self, l, xsrc, xsb):
        P, nc = self.P, self.nc
        mixT = self.scr["mixT"].rearrange("(k p) s -> p k s", p=128)
        with Scope(P) as sc:
            mbufs = [sc.tile("mt", [128, 16, TP], BF16) for _ in range(2)]
            wbufs = [sc.tile("wb", [128, 16, 512], BF16) for _ in range(3)]
            Y = [sc.tile("Y", [128, D], F32) for _ in range(4)]
            xt = Rot([sc.tile("xt", [128, D], F32) for _ in range(2)])
            self.junk = sc.tile("junk", [128, D], BF16)
            g2 = sc.tile("g2", [128, D], F32)
            smalls = Rot([[sc.tile("sm", [128, 1], F32) for _ in range(3)] for _ in range(2)])
            psrot = Rot(self.ps)
            self.load_g(g2, 1, l)

            def loadw(j, W):
                wsrc, wbuf = self.wqb[("out", l, j % 4)]
                self.ensure_cast(wbuf)
                P.dma(P.sp, W[:], wsrc, reads=[wbuf], writes=[W], sembuf=W)

            def loadm(j, M):
                P.dma(P.sp, M[:], mixT[:, :, j * TP:(j + 1) * TP], reads=[self.scrb["mixT"]], writes=[M], sembuf=M)

            wp = Pref(NPASS * 4, wbufs, loadw, 2)
            mp = Pref(NPASS, mbufs, loadm, 1)
            ei = 0
            for tp in range(NPASS):
                self.emit_casts(1)
                M = mp.get(tp)
                for cbk in range(4):
                    W = wp.get(tp * 4 + cbk)
                    for tt in range(4):
                        ps = psrot.next()
                        for k in range(16):
                            P.mm(ps[:, :], M[:, k, tt * 128:(tt + 1) * 128], W[:, k, :], k == 0, k == 15, [W, M], [ps], signal=(k == 15))
                        ysl = Y[tt][:, cbk * 512:(cbk + 1) * 512]
                        if ei % 2 == 0:
                            P.op(P.act, lambda: nc.scalar.copy(out=ysl, in_=ps[:]), [ps], [Y[tt]])
                        else:
                            P.v(lambda: nc.vector.tensor_copy(ysl, ps[:]), [ps], [Y[tt]])
                        ei += 1
                for tt in range(4):
                    t0 = tp * TP + tt * 128
                    X = xt.next()
                    P.dflush_for(X)
                    P.dma(P.sp, X[:], xsrc[t0:t0 + 128, :], reads=[self.outbs[t0 // 128]] if xsb else [], writes=[X], sembuf=X)
                    self.post_norm_residual(Y[tt], X, g2, smalls.next(), slice(t0, t0 + 128))
            P.dflush(0)

    def phase_D(self, l):
        P, nc = self.P, self.nc
        with Scope(P) as sc:
            xt = Rot([sc.tile("xt", [128, D], F32) for _ in range(2)])
            hbs = [sc.tile("hb", [128, D], BF16) for _ in range(2)]
            hbt = hbs[0]
            self.junk = hbt
            hT = [sc.tile("hT", [128, 16, TP], BF16) for _ in range(2)]
            wbufs = [sc.tile("wb", [128, 32, 128], BF16) for _ in range(2)]
            wdbufs = [sc.tile("wd", [128, 11, 512], BF16) for _ in range(2)]
            actT = sc.tile("actT", [128, 44, TP], BF16)
            Fo = [sc.tile("F", [128, D], F32) for _ in range(4)]
            sg = Rot([sc.tile("sg", [128, 512], F32) for _ in range(2)])
            g3 = sc.tile("g3", [128, D], F32)
            g4 = sc.tile("g4", [128, D], F32)
            smalls = Rot([[sc.tile("sm", [128, 1], F32) for _ in range(3)] for _ in range(2)])
            psrot = Rot(self.ps)
            self.load_g(g3, 2, l)
            self.load_g(g4, 3, l)

            def loadgu(j, W):
                hb2, cc = divmod(j % 44, 2)
                s1, b1 = self.wqb[("gate", l, hb2)]
                s2, b2 = self.wqb[("up", l, hb2)]
                self.ensure_cast(b1)
                self.ensure_cast(b2)
                if DBG.get('castbar'):
                    P.barrier()
                P.dma(P.sp, W[:, 0:16, :], s1[:, :, cc * 128:(cc + 1) * 128], reads=[b1], writes=[W], sembuf=W)
                P.dma(P.sp, W[:, 16:32, :], s2[:, :, cc * 128:(cc + 1) * 128], reads=[b2], writes=[W], sembuf=W)
                if DBG.get('dbgw') and j == DBG['dbgw'] - 1:
                    P.dma(P.sp, self.dbgw, W[:].rearrange('p k c -> p (k c)'), reads=[W], writes=[self.dbgwb], sembuf=W)

            def loadwd(j, W):
                jj = j % 16
                s1, b1 = self.wqb[("down", l, jj // 4, jj % 4)]
                self.ensure_cast(b1)
                P.dma(P.sp, W[:], s1, reads=[b1], writes=[W], sembuf=W)

            gup = Pref(NPASS * 44, wbufs, loadgu, 1)
            wdp = Pref(NPASS * 16, wdbufs, loadwd, 1)
            self.ei = 0

            def npart(tp, tt):
                t0 = tp * TP + tt * 128
                X = xt.next()
                P.dflush_for(X)
                P.dma(P.sp, X[:], self.out[t0:t0 + 128, :], reads=[self.outbs[t0 // 128]], writes=[X], sembuf=X)
                self.norm_part(X, hbs[tt % 2], g3, smalls.next())

            def tpart(tp, tt):
                self.trans_part(hbs[tt % 2], hT[tp % 2], tt, psrot, self.ei)
                self.ei += 1

            def do_norm_early(tp):
                npart(tp, 0)
                npart(tp, 1)

            def do_norm_late(tp):
                tpart(tp, 0)
                npart(tp, 2)
                tpart(tp, 1)
                npart(tp, 3)
                tpart(tp, 2)
                tpart(tp, 3)

            def do_norm(tp):
                do_norm_early(tp)
                do_norm_late(tp)

            P.dflush(0)
            do_norm(0)
            for tp in range(NPASS):
                self.emit_casts(1)
                if DBG.get('noearly') and tp > 0:
                    do_norm(tp)
                HT = hT[tp % 2]
                for hc in range(44):
                    W = gup.get(tp * 44 + hc)
                    psg = psrot.next()
                    psu = psrot.next()
                    for k in range(16):
                        P.mm(psg[:, :], W[:, k, :], HT[:, k, :], k == 0, k == 15, [W, HT], [psg], signal=(k == 15))
                    for k in range(16):
                        P.mm(psu[:, :], W[:, 16 + k, :], HT[:, k, :], k == 0, k == 15, [W, HT], [psu], signal=(k == 15))
                    sgt = sg.next()
                    P.actf(sgt[:], psg[:], AF.Silu, [psg], [sgt])
                    P.v(lambda: nc.vector.tensor_tensor(actT[:, hc, :], sgt[:], psu[:], ALU.mult), [sgt, psu], [actT])
                for cbk in range(4):
                    banks = [psrot.next() for _ in range(4)]
                    if cbk == 1 and tp + 1 < NPASS and not DBG.get('noearly'):
                        do_norm_early(tp + 1)
                    for kg in range(4):
                        Wd = wdp.get(tp * 16 + cbk * 4 + kg)
                        for tt in range(4):
                            for k in range(11):
                                P.mm(banks[tt][:, :], actT[:, kg * 11 + k, tt * 128:(tt + 1) * 128], Wd[:, k, :],
                                     kg == 0 and k == 0, kg == 3 and k == 10, [Wd, actT], [banks[tt]], signal=(k == 10))
                    for tt in range(4):
                        fsl = Fo[tt][:, cbk * 512:(cbk + 1) * 512]
                        if self.ei % 2 == 0:
                            P.op(P.act, lambda: nc.scalar.copy(out=fsl, in_=banks[tt][:]), [banks[tt]], [Fo[tt]])
                        else:
                            P.v(lambda: nc.vector.tensor_copy(fsl, banks[tt][:]), [banks[tt]], [Fo[tt]])
                        self.ei += 1
                    if cbk == 1 and tp + 1 < NPASS and not DBG.get('noearly'):
                        do_norm_late(tp + 1)
                for tt in range(4):
                    t0 = tp * TP + tt * 128
                    X = xt.next()
                    P.dflush_for(X)
                    P.dma(P.sp, X[:], self.out[t0:t0 + 128, :], reads=[self.outbs[t0 // 128]], writes=[X], sembuf=X)
                    self.post_norm_residual(Fo[tt], X, g4, smalls.next(), slice(t0, t0 + 128))
            P.dflush(0)

    def phase_B(self, l):
        P = self.P
        with Scope(P) as sc:
            self.ppt = sc.tile("ppt", [128, self.pp.shape[2]], F32)
            P.dma(P.sp, self.ppt[:], self.pp[l], writes=[self.ppt], sembuf=self.ppt)
            if "a" in self.mixers:
                self.mixer_ret(l)
            if "b" in self.mixers:
                self.mixer_ssd(l)
            if "c" in self.mixers:
                self.mixer_diff(l)
            if "d" in self.mixers:
                self.mixer_dil(l)

    def rms_feat(self, src_tiles, rows, meanname, gain_ap, gain_buf, outs, ps_pool, tmp, mult_tiles=None):
        P, nc = self.P, self.nc
        sqs, lnv, rs = tmp
        psm = ps_pool.next()
        n = len(src_tiles)
        for i, (ap, buf) in enumerate(src_tiles):
            P.actf(sqs[i][0:rows, :], ap, AF.Square, [buf], [sqs[i]])
        for i in range(n):
            P.mm(psm[0:rows, :], self.cfc(meanname, rows)[:, 0:rows], sqs[i][0:rows, :], i == 0, i == n - 1, [sqs[i], self.cf], [psm], signal=(i == n - 1))
        P.actf(lnv[0:rows, :], psm[0:rows, :], AF.Ln, [psm, self.cf], [lnv], bias=self.cfc("eps", rows), scale=1.0)
        P.actf(rs[0:rows, :], lnv[0:rows, :], AF.Exp, [lnv], [rs], scale=-0.5)
        return rs

    def mixer_ret(self, l):
        P, nc = self.P, self.nc
        qk = self.scr["r_qkT"]
        with Scope(P) as sc:
            qT = sc.tile("qT", [64, S], BF16)
            kT = sc.tile("kT", [64, S], BF16)
            qx = sc.tile("qx", [64, S], BF16)
            ktm = sc.tile("ktm", [128, NT, 64], BF16)
            kz = sc.tile("kz", [128, NT, 64], BF16)
            vt = sc.tile("vt", [128, NT, 128], BF16)
            gT = sc.tile("gT", [128, S], BF16)
            prev = sc.tile("prev", [64, NT, 128], BF16)
            Sst = sc.tile("Sst", [64, 128], F32)
            scb = Rot([sc.tile("scb", [128, 512], BF16) for _ in range(2)])
            sq = sc.tile("sq", [128, 512], F32)
            lnv = sc.tile("lnv", [128, 512], F32)
            rs = sc.tile("rs", [128, 512], F32)
            t1 = sc.tile("t1", [128, 512], F32)
            ob = Rot([sc.tile("ob", [128, 512], BF16) for _ in range(2)])
            pool = Rot(self.ps)
            for h in range(4):
                self.emit_casts(3)
                decay = float(np.exp(np.float64(math.log1p(-2.0 ** (-5.0 - h))) * 128))
                P.dma(P.sp, qT[:], qk[h * 64:(h + 1) * 64, :], reads=[self.scrb["r_qkT"]], writes=[qT], sembuf=qT)
                P.dma(P.sp, kT[:], qk[256 + h * 64:256 + (h + 1) * 64, :], reads=[self.scrb["r_qkT"]], writes=[kT], sembuf=kT)
                P.dma(P.sp, ktm[:], self.scr["r_ktm"][:, h * 64:(h + 1) * 64].rearrange("(n p) c -> p n c", p=128),
                      reads=[self.scrb["r_ktm"]], writes=[ktm], sembuf=ktm)
                P.dma(P.sp, vt[:], self.scr["r_vtm"][:, h * 128:(h + 1) * 128].rearrange("(n p) c -> p n c", p=128),
                      reads=[self.scrb["r_vtm"]], writes=[vt], sembuf=vt)
                P.dma(P.sp, gT[:], self.scr["r_gT"][h * 128:(h + 1) * 128, :], reads=[self.scrb["r_gT"]], writes=[gT], sembuf=gT)
                xi = self.cfc(f"xi{h}", 64)
                P.v(lambda: nc.vector.tensor_tensor(qx[:].rearrange("p (n c) -> p n c", c=128), qT[:].rearrange("p (n c) -> p n c", c=128),
                                                    xi.unsqueeze(1).broadcast_to([64, NT, 128]), ALU.mult), [qT, self.cf], [qx])
                P.v(lambda: nc.vector.tensor_scalar(kz[:], ktm[:], self.cfc(f"zeta{h}"), None, ALU.mult), [ktm, self.cf], [kz])
                P.v(lambda: nc.vector.memset(Sst[:], 0.0), [], [Sst])
                for n4 in range(8):
                    ps = pool.next()
                    for j in range(4):
                        n = n4 * 4 + j
                        P.mm(ps[0:64, j * 128:(j + 1) * 128], kz[:, n, :], vt[:, n, :], True, True, [kz, vt], [ps], signal=(j == 3))
                    for j in range(4):
                        n = n4 * 4 + j
                        P.v(lambda: nc.vector.tensor_copy(prev[:, n, :], Sst[:]), [Sst], [prev])
                        P.v(lambda: nc.vector.scalar_tensor_tensor(Sst[:], Sst[:], decay, ps[0:64, j * 128:(j + 1) * 128], ALU.mult, ALU.add),
                            [Sst, ps], [Sst])
                rmask = self.cfc(f"rmask{h}")
                gain = self.ppc(self.ppt, "retn")[:, h:h + 1]
                def front(st):
                    n4 = st["n4"]
                    pss = pool.next()
                    for j in range(4):
                        n = n4 * 4 + j
                        P.mm(pss[:, j * 128:(j + 1) * 128], kT[:, n * 128:(n + 1) * 128], qT[:, n * 128:(n + 1) * 128], True, True,
                             [kT, qT], [pss], signal=(j == 3))
                    scs = scb.next()
                    P.v(lambda: nc.vector.tensor_tensor(scs[:], pss[:], rmask, ALU.mult), [pss, self.cf], [scs])
                    st["scs"] = scs

                def back(st):
                    n4, scs = st["n4"], st["scs"]
                    psy = pool.next()
                    for j in range(4):
                        n = n4 * 4 + j
                        P.mm(psy[:, j * 128:(j + 1) * 128], vt[:, n, :], scs[:, j * 128:(j + 1) * 128], True, False, [vt, scs], [psy], signal=False)
                        P.mm(psy[:, j * 128:(j + 1) * 128], prev[:, n, :], qx[:, n * 128:(n + 1) * 128], False, True, [prev, qx], [psy], signal=(j == 3))
                    rsx = self.rms_feat([(psy[:], psy)], 128, "mean128", None, None, None, pool, ([sq], lnv, rs))
                    P.v(lambda: nc.vector.scalar_tensor_tensor(t1[:], psy[:], gain, rsx[:], ALU.mult, ALU.mult), [psy, self.ppt, rs], [t1])
                    o = ob.next()
                    P.v(lambda: nc.vector.tensor_tensor(o[:], t1[:], gT[:, n4 * 512:(n4 + 1) * 512], ALU.mult), [t1, gT], [o])
                    P.dma(P.sp, self.scr["mixT"][h * 128:(h + 1) * 128, n4 * 512:(n4 + 1) * 512], o[:], reads=[o], writes=[self.scrb["mixT"]], sembuf=o)

                self.pipeline([dict(n4=n4) for n4 in range(8)], front, back, 1)

    @staticmethod
    def pipeline(steps, front, back, look):
        n = len(steps)
        for i in range(min(look, n)):
            front(steps[i])
        for i in range(n):
            if i + look < n:
                front(steps[i + look])
            back(steps[i])

    def mixer_diff(self, l):
        P, nc = self.P, self.nc
        lam_init = 0.8 - 0.6 * math.exp(-0.3 * l)
        with Scope(P) as sc:
            Kc = [[sc.tile("Kc", [68, S], BF16) for _ in range(2)] for _ in range(2)]
            Qc = [[sc.tile("Qc", [68, S], BF16) for _ in range(2)] for _ in range(2)]
            V = [sc.tile("V", [128, NT, 128], BF16) for _ in range(2)]
            PT = Rot([sc.tile("PT", [128, 512], BF16) for _ in range(4)])
            R = [sc.tile("R", [128, 512], F32) for _ in range(2)]
            AB = [sc.tile("AB", [128, 512], F32) for _ in range(2)]
            Dd = sc.tile("Dd", [128, 512], F32)
            sq = sc.tile("sq", [128, 512], F32)
            lnv = sc.tile("lnv", [128, 512], F32)
            rs = sc.tile("rs", [128, 512], F32)
            ob = Rot([sc.tile("ob", [128, 512], BF16) for _ in range(2)])
            lt = sc.tile("lt", [128, 8], F32)
            lp = sc.tile("lp", [128, 128], F32)
            accp = Rot(self.ps[0:4])
            scp = Rot(self.ps[4:8])
            lamv = self.ppc(self.ppt, "lam")
            P.v(lambda: nc.vector.tensor_tensor(lp[:, 0:64], lamv[:, 0:64], lamv[:, 64:128], ALU.mult), [self.ppt], [lp])
            P.v(lambda: nc.vector.tensor_tensor(lp[:, 64:128], lamv[:, 128:192], lamv[:, 192:256], ALU.mult), [self.ppt], [lp])
            P.v(lambda: nc.vector.reduce_sum(lt[:, 0:1], lp[:, 0:64], axis=AX.X), [lp], [lt])
            P.v(lambda: nc.vector.reduce_sum(lt[:, 1:2], lp[:, 64:128], axis=AX.X), [lp], [lt])
            P.actf(lt[:, 2:4], lt[:, 0:2], AF.Exp, [lt], [lt])
            P.v(lambda: nc.vector.tensor_tensor(lt[:, 4:5], lt[:, 3:4], lt[:, 2:3], ALU.subtract), [lt], [lt])
            P.v(lambda: nc.vector.tensor_scalar(lt[:, 5:6], lt[:, 4:5], -lam_init, None, ALU.add), [lt], [lt])
            P.v(lambda: nc.vector.tensor_scalar(lt[:, 6:7], self.ppc(self.ppt, "difn"), 1.0 - lam_init, None, ALU.mult), [self.ppt], [lt])
            neglam = lt[:, 5:6]
            gain = lt[:, 6:7]
            ident = self.cbc("ident")
            ones = self.cbc("ones")
            mdiag = self.cbc("mdiag")
            NH = DBG.get('diff_nh', 4)

            def load_kq(h):
                hp = h % 2
                for c in range(2):
                    r0 = h * 128 + c * 64
                    P.dma(P.sp, Qc[hp][c][0:64, :], self.scr["d_qT"][r0:r0 + 64, :], reads=[self.scrb["d_qT"]], writes=[Qc[hp][c]], sembuf=Qc[hp][c])
                    P.dma(P.sp, Qc[hp][c][64:68, :], self.aug[h, 1], writes=[Qc[hp][c]], sembuf=Qc[hp][c])
                    P.dma(P.sp, Kc[hp][c][0:64, :], self.scr["d_kT"][r0:r0 + 64, :], reads=[self.scrb["d_kT"]], writes=[Kc[hp][c]], sembuf=Kc[hp][c])
                    P.dma(P.sp, Kc[hp][c][64:68, :], self.aug[h, 0], writes=[Kc[hp][c]], sembuf=Kc[hp][c])

            def load_v(h):
                hp = h % 2
                P.dma(P.sp, V[hp][:], self.scr["d_vtm"][:, h * 128:(h + 1) * 128].rearrange("(n p) c -> p n c", p=128),
                      reads=[self.scrb["d_vtm"]], writes=[V[hp]], sembuf=V[hp])

            steps = []
            for h in range(NH):
                for Q in range(8):
                    nJ = 4 * Q + 4
                    grp = {}
                    for J in range(nJ):
                        for c in range(2):
                            steps.append(dict(h=h, Q=Q, J=J, c=c, nJ=nJ, grp=grp, foh=(Q == 0 and J == 0 and c == 0),
                                              fog=(J == 0 and c == 0), log=(J == nJ - 1 and c == 1)))
            load_kq(0)
            load_v(0)

            def front(s):
                h, Q, J, c = s["h"], s["Q"], s["J"], s["c"]
                hp = h % 2
                if s["foh"] and h + 1 < NH:
                    load_kq(h + 1)
                if s["fog"]:
                    self.emit_casts(2)
                r = J - 4 * Q
                c0 = 128 * r if r >= 0 else 0
                ps = scp.next()
                if r >= 0:
                    P.mm(ps[:, c0:512], ident, mdiag[:, 0:512 - c0], True, False, [self.cb], [ps], signal=False)
                P.mm(ps[:, c0:512], Kc[hp][c][0:68, J * 128:(J + 1) * 128], Qc[hp][c][0:68, Q * 512 + c0:(Q + 1) * 512],
                     r < 0, True, [Kc[hp][c], Qc[hp][c]], [ps])
                pt = PT.next()
                P.actf(pt[:, c0:512], ps[:, c0:512], AF.Exp, [ps], [pt], scale=0.125)
                s["pt"] = pt
                s["c0"] = c0

            def back(s):
                h, Q, J, c, nJ, grp = s["h"], s["Q"], s["J"], s["c"], s["nJ"], s["grp"]
                hp = h % 2
                if s["foh"] and h + 1 < NH:
                    load_v(h + 1)
                if s["fog"]:
                    grp["O"] = [accp.next(), accp.next()]
                    grp["L"] = [accp.next(), accp.next()]
                O, Lb = grp["O"], grp["L"]
                pt, c0 = s["pt"], s["c0"]
                P.mm(O[c][:, c0:512], V[hp][:, J, :], pt[:, c0:512], J == 0, J == nJ - 1, [V[hp], pt], [O[c]], signal=False)
                P.mm(Lb[c][:, c0:512], ones, pt[:, c0:512], J == 0, J == nJ - 1, [self.cb, pt], [Lb[c]], signal=True)
                if s["log"]:
                    for cc in range(2):
                        P.v(lambda: nc.vector.reciprocal(R[cc][:], Lb[cc][:]), [Lb[cc]], [R[cc]])
                        P.v(lambda: nc.vector.tensor_tensor(AB[cc][:], O[cc][:], R[cc][:], ALU.mult), [O[cc], R[cc]], [AB[cc]])
                    P.v(lambda: nc.vector.scalar_tensor_tensor(Dd[:], AB[1][:], neglam, AB[0][:], ALU.mult, ALU.add), [AB[0], AB[1], lt], [Dd])
                    rsx = self.rms_feat([(Dd[:], Dd)], 128, "mean128", None, None, None, scp, ([sq], lnv, rs))
                    o = ob.next()
                    P.v(lambda: nc.vector.scalar_tensor_tensor(o[:], Dd[:], gain, rsx[:], ALU.mult, ALU.mult), [Dd, lt, rs], [o])
                    P.dma(P.sp, self.scr["mixT"][1024 + h * 128:1024 + (h + 1) * 128, Q * 512:(Q + 1) * 512], o[:],
                          reads=[o], writes=[self.scrb["mixT"]], sembuf=o)

            self.pipeline(steps, front, back, 3)

    def mixer_dil(self, l):
        P, nc = self.P, self.nc
        pats = (1, 4, 16)
        with Scope(P) as sc:
            Ka = [sc.tile("Ka", [68, S], BF16) for _ in range(2)]
            Qa = [sc.tile("Qa", [68, S], BF16) for _ in range(2)]
            Vp = [sc.tile("Vp", [128, NT, 512], BF16) for _ in range(3)]
            PT = Rot([sc.tile("PT", [128, 256], BF16) for _ in range(4)])
            Ntot = sc.tile("Ntot", [64, S], F32)
            Ltot = sc.tile("Ltot", [64, S], F32)
            ob = sc.tile("ob", [64, S], BF16)
            accp = Rot(self.ps[0:4])
            scp = Rot(self.ps[4:8])
            ident = self.cbc("ident")
            ones = self.cbc("ones")
            band = self.cbc("band")
            vb = self.scrb["l_vtm"]
            for pi, dil in enumerate(pats):
                src = self.scr["l_vtm"].rearrange("(n p r) c -> p r n c", p=128, r=dil)
                dstv = Vp[pi][:].rearrange("p (r n) c -> p r n c", r=dil)
                for r in range(dil):
                    P.dma(P.sp, dstv[:, r], src[:, r], reads=[vb], writes=[Vp[pi]], sembuf=Vp[pi])

            def load_kq(h):
                hp = h % 2
                P.dma(P.sp, Qa[hp][0:64, :], self.scr["l_qT"][h * 64:(h + 1) * 64, :], reads=[self.scrb["l_qT"]], writes=[Qa[hp]], sembuf=Qa[hp])
                P.dma(P.sp, Qa[hp][64:68, :], self.aug[4 + h, 1], writes=[Qa[hp]], sembuf=Qa[hp])
                P.dma(P.sp, Ka[hp][0:64, :], self.scr["l_kT"][h * 64:(h + 1) * 64, :], reads=[self.scrb["l_kT"]], writes=[Ka[hp]], sembuf=Ka[hp])
                P.dma(P.sp, Ka[hp][64:68, :], self.aug[4 + h, 0], writes=[Ka[hp]], sembuf=Ka[hp])

            steps = []
            for h in range(8):
                hsteps = []
                for pi, dil in enumerate(pats):
                    nb = NT // dil
                    for r in range(dil):
                        for g0 in range(0, nb, 4):
                            gsz = min(4, nb - g0)
                            ns = list(range(max(g0 - 1, 0), g0 + gsz))
                            grp = {}
                            for n in ns:
                                hsteps.append(dict(h=h, pi=pi, dil=dil, r=r, g0=g0, gsz=gsz, n=n, nb=nb, grp=grp,
                                                   fog=(n == ns[0]), log=(n == ns[-1]), foh=False, loh=False))
                hsteps[0]["foh"] = True
                hsteps[-1]["loh"] = True
                steps += hsteps
            load_kq(0)

            def front(s):
                h, dil, r, g0, gsz, n = s["h"], s["dil"], s["r"], s["g0"], s["gsz"], s["n"]
                hp = h % 2
                if s["foh"]:
                    self.emit_casts(2)
                if s["foh"] and h + 1 < 8:
                    load_kq(h + 1)
                qbs = [qb for qb in (n, n + 1) if g0 <= qb < g0 + gsz]
                lo = (qbs[0] - n) * 128
                N = 128 * len(qbs)
                ps = scp.next()
                P.mm(ps[:, 0:N], ident, band[:, lo:lo + N], True, False, [self.cb], [ps], signal=False)
                P.mm(ps[:, 0:N], Ka[hp][0:68, DS(n * 128 * dil + r, 128, dil)], Qa[hp][0:68, DS(qbs[0] * 128 * dil + r, N, dil)],
                     False, True, [Ka[hp], Qa[hp]], [ps])
                pt = PT.next()
                P.actf(pt[:, 0:N], ps[:, 0:N], AF.Exp, [ps], [pt], scale=0.125)
                s["pt"] = pt
                s["qbs"] = qbs

            def back(s):
                h, pi, dil, r, g0, gsz, n, nb, grp = s["h"], s["pi"], s["dil"], s["r"], s["g0"], s["gsz"], s["n"], s["nb"], s["grp"]
                if s["fog"]:
                    grp["On"] = accp.next()
                    grp["Ln"] = accp.next()
                On, Ln_ = grp["On"], grp["Ln"]
                pt = s["pt"]
                for qi, qb in enumerate(s["qbs"]):
                    firstc = (n == qb - 1) or (qb == 0)
                    lastc = (n == qb)
                    col = (qb - g0) * 128
                    P.mm(On[0:64, col:col + 128], Vp[pi][:, r * nb + n, h * 64:(h + 1) * 64], pt[:, qi * 128:(qi + 1) * 128],
                         firstc, lastc, [Vp[pi], pt], [On], signal=False)
                    P.mm(Ln_[0:64, col:col + 128], ones[:, 0:64], pt[:, qi * 128:(qi + 1) * 128],
                         firstc, lastc, [self.cb, pt], [Ln_], signal=True)
                if s["log"]:
                    tok = DS(g0 * 128 * dil + r, gsz * 128, dil)
                    W_ = gsz * 128
                    if pi == 0:
                        P.op(P.act, lambda: nc.scalar.copy(out=Ntot[:, tok], in_=On[0:64, 0:W_]), [On], [Ntot])
                        P.v(lambda: nc.vector.tensor_copy(Ltot[:, tok], Ln_[0:64, 0:W_]), [Ln_], [Ltot])
                    else:
                        P.v(lambda: nc.vector.tensor_tensor(Ntot[:, tok], On[0:64, 0:W_], Ntot[:, tok], ALU.add), [On, Ntot], [Ntot])
                        P.v(lambda: nc.vector.tensor_tensor(Ltot[:, tok], Ln_[0:64, 0:W_], Ltot[:, tok], ALU.add), [Ln_, Ltot], [Ltot])
                if s["loh"]:
                    P.v(lambda: nc.vector.reciprocal(Ltot[:], Ltot[:]), [Ltot], [Ltot])
                    P.v(lambda: nc.vector.tensor_tensor(ob[:], Ntot[:], Ltot[:], ALU.mult), [Ntot, Ltot], [ob])
                    P.dma(P.sp, self.scr["mixT"][1536 + h * 64:1536 + (h + 1) * 64, :], ob[:], reads=[ob], writes=[self.scrb["mixT"]], sembuf=ob)

            self.pipeline(steps, front, back, 2)

    def mixer_ssd(self, l):
        P, nc = self.P, self.nc
        ppt = self.ppt
        with Scope(P) as sc:
            z = sc.tile("z", [128, NT, 8], F32)
            az = sc.tile("az", [128, NT, 8], F32)
            dtv = sc.tile("dtv", [128, NT, 8], F32)
            a = sc.tile("a", [128, NT, 8], F32)
            acum = sc.tile("acum", [128, NT, 8], F32)
            dte = sc.tile("dte", [128, NT, 8], F32)
            cdec = sc.tile("cdec", [128, NT, 8], F32)
            wts = sc.tile("wts", [128, NT, 8], F32)
            Aneg = sc.tile("Aneg", [128, 8], F32)
            DI = sc.tile("DI", [128, 8, 128], BF16)
            pool = Rot(self.ps)
            P.dma(P.sp, z[:], self.scr["s_dt"].rearrange("(n p) h -> p n h", p=128), reads=[self.scrb["s_dt"]], writes=[z], sembuf=z)
            if DBG.get('ssd_sub') == 1:
                return
            dtb = self.ppc(ppt, "dtb")
            P.v(lambda: nc.vector.tensor_tensor(z[:], z[:], dtb.unsqueeze(1).broadcast_to([128, NT, 8]), ALU.add), [z, ppt], [z])
            if DBG.get('ssd_sub') == 2:
                return
            P.actf(az[:], z[:], AF.Abs, [z], [az])
            P.actf(az[:], az[:], AF.Exp, [az], [az], scale=-1.0)
            P.actf(az[:], az[:], AF.Ln, [az, self.cf], [az], bias=self.cfc("one"), scale=1.0)
            if DBG.get('ssd_sub') == 3:
                return
            P.v(lambda: nc.vector.scalar_tensor_tensor(dtv[:], z[:], 0.0, az[:], ALU.max, ALU.add), [z, az], [dtv])
            if DBG.get('ssd_sub') == 4:
                return
            P.actf(Aneg[:], self.ppc(ppt, "alog"), AF.Exp, [ppt], [Aneg])
            P.v(lambda: nc.vector.tensor_scalar(Aneg[:], Aneg[:], -1.0, None, ALU.mult), [Aneg], [Aneg])
            P.v(lambda: nc.vector.tensor_tensor(a[:], dtv[:], Aneg[:].unsqueeze(1).broadcast_to([128, NT, 8]), ALU.mult), [dtv, Aneg], [a])
            if DBG.get('ssd_sub') == 5:
                return
            a2 = a[:].rearrange("p n h -> p (n h)")
            ps1 = pool.next()
            P.mm(ps1[:, 0:256], self.cfc("tri"), a2, True, True, [self.cf, a], [ps1])
            P.v(lambda: nc.vector.tensor_copy(acum[:].rearrange("p n h -> p (n h)"), ps1[:, 0:256]), [ps1], [acum])
            if DBG.get('ssd_sub') == 6:
                return
            ps2 = pool.next()
            P.mm(ps2[:, 0:256], self.cfc("onesf"), a2, True, True, [self.cf, a], [ps2])
            P.actf(cdec[:].rearrange("p n h -> p (n h)"), ps2[:, 0:256], AF.Exp, [ps2], [cdec])
            P.v(lambda: nc.vector.tensor_tensor(dte[:].rearrange("p n h -> p (n h)"), ps2[:, 0:256], acum[:].rearrange("p n h -> p (n h)"), ALU.subtract),
                [ps2, acum], [dte])
            P.actf(dte[:], dte[:], AF.Exp, [dte], [dte])
            P.v(lambda: nc.vector.tensor_tensor(wts[:], dtv[:], dte[:], ALU.mult), [dtv, dte], [wts])
            if DBG.get('ssd_sub') == 7:
                return
            dsk = self.ppc(ppt, "dsk")
            for hh in range(8):
                P.v(lambda: nc.vector.tensor_scalar(DI[:, hh, :], self.cbc("ident"), dsk[:, hh:hh + 1], None, ALU.mult), [self.cb, ppt], [DI])
            if DBG.get('ssd_stop') == 1:
                return
            convw = self.ppc(ppt, "convw")
            convb = self.ppc(ppt, "convb")
            for g in range(2):
                self.emit_casts(4)
                with Scope(P) as sg:
                    BT = sg.tile("BT", [128, S], BF16)
                    CT = sg.tile("CT", [128, S], BF16)
                    xtm = sg.tile("xtm", [128, NT, 256], BF16)
                    Btm = sg.tile("Btm", [128, NT, 128], BF16)
                    with Scope(P) as s1:
                        xin = s1.tile("xin", [128, S + 3], BF16)
                        acc = s1.tile("acc", [128, S], F32)
                        xcb = s1.tile("xcb", [128, S], BF16)
                        P.v(lambda: nc.vector.memset(xin[:, 0:3], 0.0), [], [xin])
                        blocks = [(2 * g, "x0"), (2 * g + 1, "x1"), (4 + g, "B"), (6 + g, "C")]
                        for cbk, kind in blocks:
                            P.dma(P.sp, xin[:, 3:3 + S], self.scr["s_xbcT"][cbk * 128:(cbk + 1) * 128, :], reads=[self.scrb["s_xbcT"]], writes=[xin], sembuf=xin)
                            P.v(lambda: nc.vector.tensor_scalar(acc[:], xin[:, 3:3 + S], convw[:, cbk * 4 + 3:cbk * 4 + 4], convb[:, cbk:cbk + 1], ALU.mult, ALU.add),
                                [xin, ppt], [acc])
                            for k in range(3):
                                P.v(lambda: nc.vector.scalar_tensor_tensor(acc[:], xin[:, k:k + S], convw[:, cbk * 4 + k:cbk * 4 + k + 1], acc[:], ALU.mult, ALU.add),
                                    [xin, ppt, acc], [acc])
                            dstt = {"x0": xcb, "x1": xcb, "B": BT, "C": CT}[kind]
                            P.actf(dstt[:], acc[:], AF.Silu, [acc], [dstt])
                            if kind != "C":
                                for n4 in range(8):
                                    ps = pool.next()
                                    psb = ps[:].bitcast(BF16)
                                    for j in range(4):
                                        n = n4 * 4 + j
                                        P.op(P.pe, lambda: nc.tensor.transpose(psb[:, j * 128:(j + 1) * 128], dstt[:, n * 128:(n + 1) * 128], self.cbc("ident")),
                                             [dstt, self.cb], [ps], signal=(j == 3))
                                    srcv = psb[:, 0:512].rearrange("p (a b) -> p a b", a=4)
                                    if kind == "B":
                                        dv_, db_ = Btm[:, n4 * 4:(n4 + 1) * 4, :], Btm
                                    else:
                                        co = 0 if kind == "x0" else 128
                                        dv_, db_ = xtm[:, n4 * 4:(n4 + 1) * 4, co:co + 128], xtm
                                    P.v(lambda: nc.vector.tensor_copy(dv_, srcv), [ps], [db_])
                    if DBG.get('ssd_stop') == 2:
                        continue
                    with Scope(P) as s2:
                        xd = s2.tile("xd", [128, NT, 256], BF16)
                        xs = s2.tile("xs", [128, NT, 256], BF16)
                        hprev = s2.tile("hprev", [128, NT, 256], BF16)
                        Sg = s2.tile("Sg", [128, 256], F32)
                        tmpS = s2.tile("tmpS", [128, 256], F32)
                        t1 = Rot([s2.tile("t1", [128, 512], F32) for _ in range(2)])
                        dec = Rot([s2.tile("dec", [128, 512], F32) for _ in range(2)])
                        dfs = Rot([s2.tile("dfs", [128, 512], F32) for _ in range(2)])
                        MT = Rot([s2.tile("MT", [128, 4, 128], BF16) for _ in range(2)])
                        CTh = Rot([s2.tile("CTh", [128, 4, 128], BF16) for _ in range(2)])
                        yz = [s2.tile("yz", [64, 512], F32) for _ in range(4)]
                        sqs = [s2.tile("sqs", [64, 512], F32) for _ in range(4)]
                        lnv = s2.tile("lnv", [64, 512], F32)
                        rs = s2.tile("rs", [64, 512], F32)
                        zb = Rot([s2.tile("zb", [64, 512], BF16) for _ in range(4)])
                        ob = Rot([s2.tile("ob", [64, 512], BF16) for _ in range(4)])
                        accp = Rot(self.ps[0:4])
                        scp = Rot(self.ps[4:8])
                        h4 = slice(4 * g, 4 * g + 4)
                        xv = xtm[:].rearrange("p n (h c) -> p n h c", h=4)
                        P.v(lambda: nc.vector.tensor_tensor(xd[:].rearrange("p n (h c) -> p n h c", h=4), xv,
                                                            dtv[:, :, h4].unsqueeze(3).broadcast_to([128, NT, 4, 64]), ALU.mult), [xtm, dtv], [xd])
                        P.v(lambda: nc.vector.tensor_tensor(xs[:].rearrange("p n (h c) -> p n h c", h=4), xv,
                                                            wts[:, :, h4].unsqueeze(3).broadcast_to([128, NT, 4, 64]), ALU.mult), [xtm, wts], [xs])
                        P.v(lambda: nc.vector.memset(Sg[:], 0.0), [], [Sg])
                        for n in range(NT):
                            ps = scp.next()
                            P.mm(ps[:, 0:256], Btm[:, n, :], xs[:, n, :], True, True, [Btm, xs], [ps])
                            P.v(lambda: nc.vector.tensor_copy(hprev[:, n, :], Sg[:]), [Sg], [hprev])
                            P.v(lambda: nc.vector.tensor_tensor(tmpS[:].rearrange("p (h c) -> p h c", h=4), Sg[:].rearrange("p (h c) -> p h c", h=4),
                                                                cdec[:, n, h4].unsqueeze(2).broadcast_to([128, 4, 64]), ALU.mult), [Sg, cdec], [tmpS])
                            P.v(lambda: nc.vector.tensor_tensor(Sg[:], tmpS[:], ps[:, 0:256], ALU.add), [tmpS, ps], [Sg])
                        gn = self.ppc(ppt, "ssdn")
                        steps = []
                        for n4 in range(0 if DBG.get('ssd_stop') == 3 else 8):
                            grp = {}
                            for j in range(4):
                                steps.append(dict(n4=n4, j=j, n=n4 * 4 + j, grp=grp))

                        def front(st):
                            n = st["n"]
                            psa = scp.next()
                            for hh in range(4):
                                P.mm(psa[:, hh * 128:(hh + 1) * 128], a[:, n, 4 * g + hh:4 * g + hh + 1].broadcast_to([128, 128]), self.cfc("tri"),
                                     True, True, [a, self.cf], [psa], signal=(hh == 3))
                            pscb = scp.next()
                            P.mm(pscb[:, 0:128], BT[:, n * 128:(n + 1) * 128], CT[:, n * 128:(n + 1) * 128], True, True, [BT, CT], [pscb])
                            t1t = t1.next()
                            P.v(lambda: nc.vector.tensor_tensor(t1t[:], psa[:], self.cfc("negmask4"), ALU.add), [psa, self.cf], [t1t])
                            P.v(lambda: nc.vector.tensor_tensor(t1t[:].rearrange("p (h c) -> p h c", h=4), t1t[:].rearrange("p (h c) -> p h c", h=4),
                                                                acum[:, n, h4].unsqueeze(2).broadcast_to([128, 4, 128]), ALU.subtract), [t1t, acum], [t1t])
                            dect = dec.next()
                            P.actf(dect[:], t1t[:], AF.Exp, [t1t], [dect])
                            mt = MT.next()
                            P.v(lambda: nc.vector.tensor_tensor(mt[:], dect[:].rearrange("p (h c) -> p h c", h=4),
                                                                pscb[:, 0:128].unsqueeze(1).broadcast_to([128, 4, 128]), ALU.mult), [dect, pscb], [mt])
                            dfst = dfs.next()
                            P.actf(dfst[:], psa[:], AF.Exp, [psa], [dfst])
                            cth = CTh.next()
                            P.v(lambda: nc.vector.tensor_tensor(cth[:], dfst[:].rearrange("p (h c) -> p h c", h=4),
                                                                CT[:, n * 128:(n + 1) * 128].unsqueeze(1).broadcast_to([128, 4, 128]), ALU.mult), [dfst, CT], [cth])
                            st["mt"] = mt
                            st["cth"] = cth

                        def back(st):
                            n4, j, n, grp = st["n4"], st["j"], st["n"], st["grp"]
                            if j == 0:
                                grp["y"] = [accp.next() for _ in range(4)]
                            ybank = grp["y"]
                            mt, cth = st["mt"], st["cth"]
                            for hh in range(4):
                                yo = ybank[hh][0:64, j * 128:(j + 1) * 128]
                                P.mm(yo, xd[:, n, hh * 64:(hh + 1) * 64], mt[:, hh, :], True, False, [xd, mt], [ybank[hh]], signal=False)
                                P.mm(yo, hprev[:, n, hh * 64:(hh + 1) * 64], cth[:, hh, :], False, False, [hprev, cth], [ybank[hh]], signal=False)
                                P.mm(yo, xtm[:, n, hh * 64:(hh + 1) * 64], DI[:, 4 * g + hh, :], False, True, [xtm, DI], [ybank[hh]], signal=True)
                            if j == 3:
                                srcs = []
                                for hh in range(4):
                                    hd = 4 * g + hh
                                    zt = zb.next()
                                    P.dma(P.sp, zt[:], self.scr["s_zT"][hd * 64:(hd + 1) * 64, n4 * 512:(n4 + 1) * 512], reads=[self.scrb["s_zT"]], writes=[zt], sembuf=zt)
                                    P.v(lambda: nc.vector.tensor_tensor(yz[hh][:], ybank[hh][0:64, :], zt[:], ALU.mult), [ybank[hh], zt], [yz[hh]])
                                    srcs.append((yz[hh][:], yz[hh]))
                                rsx = self.rms_feat(srcs, 64, "mean256", None, None, None, scp, (sqs, lnv, rs))
                                for hh in range(4):
                                    hd = 4 * g + hh
                                    o = ob.next()
                                    P.v(lambda: nc.vector.scalar_tensor_tensor(o[:], yz[hh][:], gn[0:64, hd:hd + 1], rsx[0:64, :], ALU.mult, ALU.mult),
                                        [yz[hh], ppt, rs], [o])
                                    P.dma(P.sp, self.scr["mixT"][512 + hd * 64:512 + (hd + 1) * 64, n4 * 512:(n4 + 1) * 512], o[:],
                                          reads=[o], writes=[self.scrb["mixT"]], sembuf=o)

                        self.pipeline(steps, front, back, 1)


_CACHE = {}


def _get_prog(key, **kw):
    if key not in _CACHE:
        _CACHE[key] = K(**kw)
    return _CACHE[key]


def make_in_maps(inputs, ncores):
    pp = _pack_params(inputs)
    maps = []
    for b in range(ncores):
        m = {
            "x": np.ascontiguousarray(inputs["x"][b]),
            "w_in": inputs["w_in"], "w_out": inputs["w_out"], "w_gate": inputs["w_gate"],
            "w_up": inputs["w_up"], "w_down": inputs["w_down"],
            "norm_mix_pre": inputs["norm_mix_pre"], "norm_mix_post": inputs["norm_mix_post"],
            "norm_ffn_pre": inputs["norm_ffn_pre"], "norm_ffn_post": inputs["norm_ffn_post"],
            "pp": pp, "cfa": CFA, "cba": CBA, "aug": AUG,
        }
        maps.append(m)
    return maps


def kernel(**inputs):
    inputs = {k: np.asarray(v) for k, v in inputs.items()}
    _pack_params(inputs)
    prog = _get_prog("full", layers=list(range(NL)))
    maps = make_in_maps(inputs, 8)
    res = run_bass_kernel_spmd(prog.nc, maps, core_ids=list(range(8)))
    return np.stack([np.asarray(r["out"], dtype=np.float32) for r in res.results], 0)
```
